# Optimizing a Trainium2 kernel written in Bass

```python
import math
import jax, jax.numpy as jnp
from jax import lax
import numpy as np

D_MODEL = 2048
BATCH = 4
SEQ = 4096
DEPTH = 1

GRID_W = 64
CTX_LEN = 256
MIX_WIDTH = D_MODEL
FOURIER_WIDTH = MIX_WIDTH // 2
FOURIER_GROUPS = 8
FOURIER_GROUP_DIM = FOURIER_WIDTH // FOURIER_GROUPS
GDN_WIDTH = MIX_WIDTH - FOURIER_WIDTH
GDN_HEADS = 8
GDN_HEAD_DIM = GDN_WIDTH // GDN_HEADS
CONV_K = 5
CHUNK = 64
N_DIR = 2
IN_COLS = FOURIER_WIDTH + 4 * GDN_WIDTH + 2 * N_DIR * GDN_HEADS
N_EXPERTS = 16
CAPACITY_FACTOR = 2
EXPERT_FF = 3 * D_MODEL // 4
EPS = 1e-6

kernel_name = 'hybrid_fourier_gdn_ecmoe_dit'


def rmsnorm(x, g):
    xf = x.astype(jnp.float32)
    y = xf * lax.rsqrt(jnp.mean(xf * xf, axis=-1, keepdims=True) + EPS)
    return y.astype(x.dtype) * g


def l2norm(t):
    tf = t.astype(jnp.float32)
    return tf * lax.rsqrt(jnp.sum(tf * tf, axis=-1, keepdims=True) + EPS)


def modulate(x, g, shift, scale):
    return rmsnorm(x, g) * (1 + scale) + shift


def short_conv(x, w):
    half = CONV_K // 2
    L = x.shape[2]
    xp = jnp.pad(x, ((0, 0), (0, 0), (half, half), (0, 0)))
    return sum(xp[:, :, j:j + L] * w[j] for j in range(CONV_K))


def fourier_mix(f):
    B, N, _ = f.shape
    fg = f.reshape(B, N, FOURIER_GROUPS, FOURIER_GROUP_DIM).astype(jnp.float32)
    y = jnp.fft.fft2(fg, axes=(1, 3), norm='ortho').real
    return y.reshape(B, N, FOURIER_WIDTH).astype(f.dtype)


def gated_delta_chunked(q, k, v, g, beta, s0):
    B, T, H, K = q.shape
    V = v.shape[-1]
    n = T // CHUNK

    def blocks(t):
        t = t.astype(jnp.float32).reshape((B, n, CHUNK) + t.shape[2:])
        return jnp.moveaxis(t, 2, 3)

    q, k, v, g, beta = (blocks(t) for t in (q, k, v, g, beta))
    gc = jnp.cumsum(g, axis=-1)
    lower = jnp.tril(jnp.ones((CHUNK, CHUNK), bool))
    strict = jnp.tril(jnp.ones((CHUNK, CHUNK), bool), -1)
    diff = gc[..., :, None] - gc[..., None, :]
    decay = jnp.where(lower, jnp.exp(jnp.where(lower, diff, 0.0)), 0.0)
    kb = k * beta[..., None]
    m = jnp.where(strict, jnp.einsum('bnhik,bnhjk->bnhij', kb, k) * decay, 0.0)
    eye = jnp.eye(CHUNK, dtype=jnp.float32)
    t_inv = lax.linalg.triangular_solve(m + eye, jnp.broadcast_to(eye, m.shape),
                                        left_side=True, lower=True, unit_diagonal=True)
    u = jnp.einsum('bnhij,bnhjv->bnhiv', t_inv, v * beta[..., None])
    w = jnp.einsum('bnhij,bnhjk->bnhik', t_inv, kb * jnp.exp(gc)[..., None])
    qk = jnp.einsum('bnhik,bnhjk->bnhij', q, k) * decay
    q_dec = q * jnp.exp(gc)[..., None]
    k_dec = k * jnp.exp(gc[..., -1:] - gc)[..., None]
    g_tot = jnp.exp(gc[..., -1])

    def step(s, xs):
        u_c, w_c, qk_c, qd_c, kd_c, gt_c = xs
        v_new = u_c - jnp.einsum('bhck,bhkv->bhcv', w_c, s)
        o_c = jnp.einsum('bhck,bhkv->bhcv', qd_c, s) + jnp.einsum('bhij,bhjv->bhiv', qk_c, v_new)
        s = s * gt_c[..., None, None] + jnp.einsum('bhck,bhcv->bhkv', kd_c, v_new)
        return s, o_c

    xs = tuple(jnp.moveaxis(t, 1, 0) for t in (u, w, qk, q_dec, k_dec, g_tot))
    s_final, o = lax.scan(step, s0.astype(jnp.float32), xs)
    o = jnp.transpose(o, (1, 0, 3, 2, 4)).reshape(B, T, H, V)
    return o, s_final


def bidirectional_gated_delta(q, k, v, a, b, a_log, dt_bias, s0_fwd, s0_bwd):
    g = -jnp.exp(a_log.astype(jnp.float32)) * jax.nn.softplus(a.astype(jnp.float32) + dt_bias.astype(jnp.float32))
    beta = jax.nn.sigmoid(b.astype(jnp.float32))
    o_f, s_f = gated_delta_chunked(q, k, v, g[:, :, 0], beta[:, :, 0], s0_fwd)
    rev = lambda t: jnp.flip(t, axis=1)
    o_b, s_b = gated_delta_chunked(rev(q), rev(k), rev(v), rev(g[:, :, 1]), rev(beta[:, :, 1]), s0_bwd)
    return o_f + rev(o_b), s_f, s_b


def token_mixer(h, w_in, conv_w, a_log, dt_bias, gdn_norm_w, w_out, s0_fwd, s0_bwd, n_rows, with_output):
    B, N, _ = h.shape
    p = h @ w_in
    cuts = [FOURIER_WIDTH, FOURIER_WIDTH + 3 * GDN_WIDTH, FOURIER_WIDTH + 4 * GDN_WIDTH,
            FOURIER_WIDTH + 4 * GDN_WIDTH + N_DIR * GDN_HEADS]
    f_in, qkv, z, a, b = jnp.split(p, cuts, axis=-1)
    qkv = jax.nn.silu(short_conv(qkv.reshape(B, n_rows, N // n_rows, 3 * GDN_WIDTH), conv_w))
    qkv = qkv.reshape(B, N, 3, GDN_HEADS, GDN_HEAD_DIM)
    q = l2norm(qkv[:, :, 0]) * GDN_HEAD_DIM ** -0.5
    k = l2norm(qkv[:, :, 1])
    v = qkv[:, :, 2]
    o, s_f, s_b = bidirectional_gated_delta(q, k, v, a.reshape(B, N, N_DIR, GDN_HEADS),
                                            b.reshape(B, N, N_DIR, GDN_HEADS), a_log, dt_bias, s0_fwd, s0_bwd)
    if not with_output:
        return None, s_f, s_b
    zg = jax.nn.silu(z.astype(jnp.float32).reshape(B, N, GDN_HEADS, GDN_HEAD_DIM))
    o = (rmsnorm(o, gdn_norm_w.astype(jnp.float32)) * zg).astype(h.dtype)
    out = jnp.concatenate([fourier_mix(f_in), o.reshape(B, N, GDN_WIDTH)], axis=-1) @ w_out
    return out, s_f, s_b


def expert_choice_ffn(h, w_router, w_gate, w_up, w_down):
    B, N, D = h.shape
    cap = CAPACITY_FACTOR * N // N_EXPERTS
    aff = jax.nn.softmax((h @ w_router).astype(jnp.float32), axis=-1)
    vals, idx = lax.top_k(jnp.swapaxes(aff, 1, 2), cap)
    xg = jax.vmap(lambda hb, ib: hb[ib])(h, idx)
    hid = jax.nn.silu(jnp.einsum('becd,edf->becf', xg, w_gate)) * jnp.einsum('becd,edf->becf', xg, w_up)
    y = jnp.einsum('becf,efd->becd', hid, w_down) * vals[..., None].astype(h.dtype)
    return jax.vmap(lambda yb, ib: jnp.zeros((N, D), yb.dtype).at[ib.reshape(-1)].add(yb.reshape(-1, D)))(y, idx)


def setup_inputs(seed: int = 0) -> dict:
    key = jax.random.key(seed)
    ks = jax.random.split(key, 20)
    f32 = jnp.float32
    D = D_MODEL

    def nrm(k, shape, scale):
        return jax.random.normal(k, shape, f32) * scale

    dt = jnp.exp(jax.random.uniform(ks[10], (DEPTH, N_DIR, GDN_HEADS), f32, math.log(1e-3), math.log(1e-1)))
    return {
        'x': nrm(ks[0], (BATCH, SEQ, D), 1.0),
        'c': nrm(ks[1], (BATCH, D), 1.0),
        'ctx': nrm(ks[2], (BATCH, CTX_LEN, D), 1.0),
        'c_ctx': nrm(ks[3], (D,), 1.0),
        'w_mod': nrm(ks[4], (DEPTH, D, 6 * D), D ** -0.5),
        'b_mod': nrm(ks[5], (DEPTH, 6 * D), 0.01),
        'norm1_g': 1.0 + nrm(ks[6], (DEPTH, D), 0.02),
        'norm2_g': 1.0 + nrm(ks[7], (DEPTH, D), 0.02),
        'w_in': nrm(ks[8], (DEPTH, D, IN_COLS), D ** -0.5),
        'conv_w': nrm(ks[9], (DEPTH, CONV_K, 3 * GDN_WIDTH), CONV_K ** -0.5),
        'a_log': jnp.log(jax.random.uniform(ks[11], (DEPTH, N_DIR, GDN_HEADS), f32, 1.0, 16.0)),
        'dt_bias': dt + jnp.log(-jnp.expm1(-dt)),
        'gdn_norm_w': 1.0 + nrm(ks[12], (DEPTH, GDN_HEAD_DIM), 0.02),
        'w_out': nrm(ks[13], (DEPTH, MIX_WIDTH, D), MIX_WIDTH ** -0.5),
        'w_router': nrm(ks[14], (DEPTH, D, N_EXPERTS), D ** -0.5),
        'w_gate': nrm(ks[15], (DEPTH, N_EXPERTS, D, EXPERT_FF), D ** -0.5),
        'w_up': nrm(ks[16], (DEPTH, N_EXPERTS, D, EXPERT_FF), D ** -0.5),
        'w_down': nrm(ks[17], (DEPTH, N_EXPERTS, EXPERT_FF, D), EXPERT_FF ** -0.5),
        'norm_f': 1.0 + nrm(ks[18], (D,), 0.02),
    }


def reference(x, c, ctx, c_ctx, w_mod, b_mod, norm1_g, norm2_g, w_in, conv_w, a_log, dt_bias,
              gdn_norm_w, w_out, w_router, w_gate, w_up, w_down, norm_f):
    B, N, _ = x.shape
    rows = N // GRID_W
    zero_state = jnp.zeros((B, GDN_HEADS, GDN_HEAD_DIM, GDN_HEAD_DIM), jnp.float32)
    for i in range(DEPTH):
        last = i == DEPTH - 1
        sh1_x, sc1_x, gt1_x, sh2_x, sc2_x, gt2_x = jnp.split(
            (jax.nn.silu(c) @ w_mod[i] + b_mod[i])[:, None, :], 6, axis=-1)
        sh1_c, sc1_c, gt1_c, sh2_c, sc2_c, gt2_c = jnp.split(
            jax.nn.silu(c_ctx) @ w_mod[i] + b_mod[i], 6, axis=-1)
        mix_w = (w_in[i], conv_w[i], a_log[i], dt_bias[i], gdn_norm_w[i], w_out[i])
        hc = modulate(ctx, norm1_g[i], sh1_c, sc1_c)
        ctx_mix, s_f, s_b = token_mixer(hc, *mix_w, zero_state, zero_state, 1, not last)
        hx = modulate(x, norm1_g[i], sh1_x, sc1_x)
        x_mix, _, _ = token_mixer(hx, *mix_w, s_f, s_b, rows, True)
        x = x + gt1_x * x_mix
        hx = modulate(x, norm2_g[i], sh2_x, sc2_x)
        x = x + gt2_x * expert_choice_ffn(hx, w_router[i], w_gate[i], w_up[i], w_down[i])
        if not last:
            ctx = ctx + gt1_c * ctx_mix
            hc = modulate(ctx, norm2_g[i], sh2_c, sc2_c)
            ctx = ctx + gt2_c * expert_choice_ffn(hc, w_router[i], w_gate[i], w_up[i], w_down[i])
    return rmsnorm(x, norm_f)
```

```python
import numpy as np
from contextlib import ExitStack
import concourse.bass as bass
import concourse.mybir as mybir

F32 = mybir.dt.float32
BF16 = mybir.dt.bfloat16
F16 = mybir.dt.float16
I32 = mybir.dt.int32
U32 = mybir.dt.uint32
AF = mybir.ActivationFunctionType
ALU = mybir.AluOpType
AX = mybir.AxisListType

ENGS = ("pe", "act", "dve", "pool", "sp")
N_DMA_SEMS = 48


class Sched:
    def __init__(self, nc, es):
        self.nc = nc
        self.streams = {e: [] for e in ENGS}
        self.esem = {e: es.enter_context(nc.semaphore("s_" + e)) for e in ENGS if e != "sp"}
        self.dsem = [es.enter_context(nc.semaphore("d%d" % i)) for i in range(N_DMA_SEMS)]
        self.count = {e: 0 for e in ENGS}
        self.dcount = [0] * N_DMA_SEMS
        self.dnext = 0
        self.waited = {e: {} for e in ENGS}
        self.res = {}
        self.same_engine_waits = True

    def _sem(self, key):
        return self.esem[key[1]] if key[0] == "e" else self.dsem[key[1]]

    def _deps(self, reads, writes):
        deps = {}
        def add(tok):
            if tok is None:
                return
            k, v = tok
            if deps.get(k, 0) < v:
                deps[k] = v
        for r in reads:
            st = self.res.get(r)
            if st:
                add(st["w"])
        for w in writes:
            st = self.res.get(w)
            if st:
                add(st["w"])
                for k, v in st["r"].items():
                    add((k, v))
        return deps

    def _update(self, reads, writes, tok):
        for r in reads:
            st = self.res.setdefault(r, {"w": None, "r": {}})
            k, v = tok
            if st["r"].get(k, 0) < v:
                st["r"][k] = v
        for w in writes:
            self.res[w] = {"w": tok, "r": {}}

    def _waits(self, eng, deps):
        ws = []
        for k, v in deps.items():
            if k == ("e", eng) and (eng == "pe" or not self.same_engine_waits):
                continue
            if self.waited[eng].get(k, 0) < v:
                self.waited[eng][k] = v
                ws.append((k, v))
        return ws

    def op(self, eng, fn, reads=(), writes=()):
        deps = self._deps(reads, writes)
        ws = self._waits(eng, deps)
        self.count[eng] += 1
        tok = (("e", eng), self.count[eng])
        self.streams[eng].append((ws, fn, tok[0], 1))
        self._update(reads, writes, tok)

    def dma(self, eng, fn, reads=(), writes=()):
        deps = self._deps(reads, writes)
        i = self.dnext
        self.dnext = (self.dnext + 1) % N_DMA_SEMS
        if self.dcount[i] > 0:
            k = ("d", i)
            if deps.get(k, 0) < self.dcount[i]:
                deps[k] = self.dcount[i]
        ws = self._waits(eng, deps)
        self.dcount[i] += 16
        tok = (("d", i), self.dcount[i])
        self.streams[eng].append((ws, fn, tok[0], 16))
        self._update(reads, writes, tok)
        return tok

    def barrier(self):
        deps = {}
        for e in ENGS:
            if e != "sp" and self.count[e] > 0:
                deps[("e", e)] = self.count[e]
        for i in range(N_DMA_SEMS):
            if self.dcount[i] > 0:
                deps[("d", i)] = self.dcount[i]
        for e in ENGS:
            ws = []
            for k, v in deps.items():
                if k == ("e", e):
                    continue
                if self.waited[e].get(k, 0) < v:
                    self.waited[e][k] = v
                    ws.append((k, v))
            self.streams[e].append((ws, None, None, 0))

    def final_wait(self, eng, resources):
        deps = self._deps(resources, resources)
        ws = self._waits(eng, deps)
        self.streams[eng].append((ws, None, None, 0))

    def emit(self):
        nc = self.nc
        with nc.Block() as block:
            def mk(name):
                def body(e):
                    for ws, fn, key, inc in self.streams[name]:
                        for k, v in ws:
                            e.wait_ge(self._sem(k), v)
                        if fn is not None:
                            ins = fn(e)
                            ins.then_inc(self._sem(key), inc)
                return body
            block.tensor(mk("pe"))
            block.scalar(mk("act"))
            block.vector(mk("dve"))
            block.gpsimd(mk("pool"))
            block.sync(mk("sp"))


D = 2048
KC = 16
NX = 4096
NCTX = 256
NT = NX + NCTX
IN_COLS = 5152
EPS = 1e-6


def bc(ap_row, n, parts=128):
    return bass.AP(ap_row.tensor, ap_row.offset, [[0, parts], [1, n]])


class Ctx:
    pass


def declare(nc, dbg=()):
    G = Ctx()
    G.nc = nc
    def inp(name, shape, dt=F32):
        return nc.dram_tensor(name, list(shape), dt, kind="ExternalInput").ap()
    def scr(name, shape, dt):
        kind = "ExternalOutput" if name in dbg else "Internal"
        return nc.dram_tensor(name, list(shape), dt, kind=kind).ap()
    G.x = inp("x", [NX, D])
    G.ctx = inp("ctx", [NCTX, D])
    G.ccT = inp("ccT", [128, KC, 2])
    G.w_mod = inp("w_mod", [D, 6 * D])
    G.b_mod = inp("b_mod", [1, 6 * D])
    G.norm1_g = inp("norm1_g", [1, D])
    G.norm2_g = inp("norm2_g", [1, D])
    G.w_in = inp("w_in", [D, IN_COLS])
    G.ident_bf = inp("ident_bf", [128, 128], BF16)
    G.out = nc.dram_tensor("out", [NX, D], F32, kind="ExternalOutput").ap()
    G.mod_d = scr("mod_d", [2, 6 * D], F32)
    G.hT_d = scr("hT_d", [D, NT], BF16)
    return G


def phase0_mod(G, S, es):
    nc = G.nc
    cT = es.enter_context(nc.sbuf_tensor("cT", [128, KC, 2], F32))
    sT = es.enter_context(nc.sbuf_tensor("sT", [128, KC, 2], BF16))
    wm = [es.enter_context(nc.sbuf_tensor("wm%d" % i, [128, KC, 512], BF16)) for i in range(2)]
    bm = [es.enter_context(nc.sbuf_tensor("bm%d" % i, [2, 512], F32)) for i in range(2)]
    gg = [es.enter_context(nc.sbuf_tensor("gg%d" % i, [2, 512], F32)) for i in range(2)]
    row = [es.enter_context(nc.sbuf_tensor("row%d" % i, [2, 512], F32)) for i in range(2)]
    S.dma("sp", lambda e: e.dma_start(out=cT[:], in_=G.ccT), writes=["cT"])
    S.op("act", lambda e: e.activation(out=sT[:], in_=cT[:], func=AF.Silu), reads=["cT"], writes=["sT"])
    wv = G.w_mod.rearrange("(k p) n -> p k n", p=128)
    for j in range(24):
        s = j % 2
        sec = j // 4
        c0 = j * 512
        S.dma("pool", lambda e, s=s, c0=c0: e.dma_start(out=wm[s][:], in_=wv[:, :, c0:c0 + 512]),
              writes=[("wm", s)])
        S.dma("sp", lambda e, s=s, c0=c0: e.dma_start(out=bm[s][:], in_=bc(G.b_mod[0:1, c0:c0 + 512], 512, 2)),
              writes=[("bm", s)])
        if sec in (1, 4):
            gsrc = G.norm1_g if sec == 1 else G.norm2_g
            g0 = c0 - sec * D
            S.dma("sp", lambda e, s=s, g0=g0, gsrc=gsrc: e.dma_start(out=gg[s][:], in_=bc(gsrc[0:1, g0:g0 + 512], 512, 2)),
                  writes=[("gg", s)])
        pb = G.ps[j % 2]
        for k in range(KC):
            S.op("pe", lambda e, s=s, k=k, pb=pb: e.matmul(pb[0:2, :], lhsT=sT[:, k, :], rhs=wm[s][:, k, :],
                                                       start=(k == 0), stop=(k == KC - 1)),
                 reads=["sT", ("wm", s)], writes=[("ps", j % 2)])
        S.op("dve", lambda e, s=s, pb=pb: e.tensor_tensor(out=row[s][:], in0=pb[0:2, :], in1=bm[s][:], op=ALU.add),
             reads=[("ps", j % 2), ("bm", s)], writes=[("row", s)])
        if sec in (1, 4):
            S.op("dve", lambda e, s=s: e.scalar_tensor_tensor(out=row[s][:], in0=row[s][:], scalar=1.0, in1=gg[s][:],
                                                              op0=ALU.add, op1=ALU.mult),
                 reads=[("row", s), ("gg", s)], writes=[("row", s)])
        S.dma("act", lambda e, s=s, c0=c0: e.dma_start(out=G.mod_d[:, c0:c0 + 512], in_=row[s][:]),
              reads=[("row", s)], writes=["mod_d"])
        yield


def phaseA_modulate(G, S, es):
    nc = G.nc
    gm = {}
    for nm in ("gmx", "shx", "gmc", "shc"):
        gm[nm] = es.enter_context(nc.sbuf_tensor(nm, [128, D], F32))
    S.dma("sp", lambda e: e.dma_start(out=gm["shx"][:], in_=bc(G.mod_d[0:1, 0:D], D)), reads=["mod_d"], writes=["shx"])
    S.dma("sp", lambda e: e.dma_start(out=gm["gmx"][:], in_=bc(G.mod_d[0:1, D:2 * D], D)), reads=["mod_d"], writes=["gmx"])
    S.dma("sp", lambda e: e.dma_start(out=gm["shc"][:], in_=bc(G.mod_d[1:2, 0:D], D)), reads=["mod_d"], writes=["shc"])
    S.dma("sp", lambda e: e.dma_start(out=gm["gmc"][:], in_=bc(G.mod_d[1:2, D:2 * D], D)), reads=["mod_d"], writes=["gmc"])
    xt = [es.enter_context(nc.sbuf_tensor("xt%d" % i, [128, D], F32)) for i in range(4)]
    h1 = [es.enter_context(nc.sbuf_tensor("h1_%d" % i, [128, D], F32)) for i in range(2)]
    hb = [es.enter_context(nc.sbuf_tensor("hb%d" % i, [128, D], BF16)) for i in range(2)]
    junk = es.enter_context(nc.sbuf_tensor("junkA", [128, D], BF16))
    ss = es.enter_context(nc.sbuf_tensor("ssA", [128, 64], F32))
    hTb = [es.enter_context(nc.sbuf_tensor("hTb%d" % i, [128, KC, 512], BF16)) for i in range(2)]
    ident = G.ident
    hv = G.hT_d.rearrange("(k p) t -> p k t", p=128)
    ntiles = NT // 128

    def info(t):
        isctx = t < 2
        if isctx:
            return isctx, 0, t, 256
        return isctx, 1 + (t - 2) // 4, (t - 2) % 4, 512

    def a_load(t):
        s3 = t % 4
        src = G.ctx[t * 128:(t + 1) * 128, :] if t < 2 else G.x[(t - 2) * 128:(t - 1) * 128, :]
        S.dma("sp", lambda e: e.dma_start(out=xt[s3][:], in_=src), writes=[("xt", s3)])

    def a_stat(t):
        s3 = t % 4
        S.op("act", lambda e: e.activation(out=junk[:], in_=xt[s3][:], func=AF.Square, scale=D ** -0.5, accum_out=ss[:, t:t + 1]),
             reads=[("xt", s3)], writes=["junkA", ("ssA", t)])
        S.op("act", lambda e: e.activation(out=ss[:, t:t + 1], in_=ss[:, t:t + 1], func=AF.Sqrt, bias=EPS, scale=1.0), reads=[("ssA", t)], writes=[("ssA", t)])
        S.op("dve", lambda e: e.reciprocal(out=ss[:, t:t + 1], in_=ss[:, t:t + 1]), reads=[("ssA", t)], writes=[("ssA", t)])

    def a_mod(t):
        s3 = t % 4; s = t % 2
        isctx = t < 2
        g_, s_ = ("gmc", "shc") if isctx else ("gmx", "shx")
        S.op("dve", lambda e: e.scalar_tensor_tensor(out=h1[s][:], in0=xt[s3][:], scalar=ss[:, t:t + 1], in1=gm[g_][:], op0=ALU.mult, op1=ALU.mult),
             reads=[("xt", s3), ("ssA", t), g_], writes=[("h1", s)])
        S.op("pool", lambda e: e.tensor_tensor(out=hb[s][:], in0=h1[s][:], in1=gm[s_][:], op=ALU.add), reads=[("h1", s), s_], writes=[("hb", s)])

    def a_tr(t):
        s = t % 2
        isctx, bi, tt, bw = info(t)
        bs = bi % 2
        for half in range(2):
            pt = G.pst[half]
            for j in range(8):
                k = half * 8 + j
                S.op("pe", lambda e, k=k, j=j, pt=pt: e.transpose(out=pt[:, j * 128:(j + 1) * 128], in_=hb[s][:, k * 128:(k + 1) * 128], identity=ident[:]),
                     reads=[("hb", s), "ident"], writes=[("ps", 6 + half)])
            o = hTb[bs][:, half * 8:(half + 1) * 8, tt * 128:(tt + 1) * 128]
            i_ = pt[:, :].rearrange("p (k t) -> p k t", k=8)
            if half == 0:
                S.op("act", lambda e, o=o, i_=i_: e.activation(out=o, in_=i_, func=AF.Copy), reads=[("ps", 6)], writes=[("hTb", bs, tt, 0)])
            else:
                S.op("dve", lambda e, o=o, i_=i_: e.tensor_copy(out=o, in_=i_), reads=[("ps", 7)], writes=[("hTb", bs, tt, 1)])
        last = (isctx and tt == 1) or ((not isctx) and tt == 3)
        if last:
            t0 = 0 if isctx else NCTX + (bi - 1) * 512
            S.dma("sp", lambda e: e.dma_start(out=hv[:, :, t0:t0 + bw], in_=hTb[bs][:, :, 0:bw]),
                  reads=[("hTb", bs, a_, b_) for a_ in range(4) for b_ in range(2)], writes=[("hT_d", bi)])

    stages = [(0, a_load), (2, a_stat), (3, a_mod), (4, a_tr)]
    for i in range(ntiles + 4):
        for k_, st in stages:
            j = i - k_
            if 0 <= j < ntiles:
                st(j)
        yield


HD = 128
NH = 8
GW = 1024
TBLKS = [(0, 256)] + [(NCTX + i * 512, 512) for i in range(8)]


def declareB(G, nc, dbg=()):
    def inp(name, shape, dt=F32):
        return nc.dram_tensor(name, list(shape), dt, kind="ExternalInput").ap()
    def scr(name, shape, dt):
        kind = "ExternalOutput" if name in dbg else "Internal"
        return nc.dram_tensor(name, list(shape), dt, kind=kind).ap()
    G.conv_wT = inp("conv_wT", [128, 24, 5])
    G.ab_par = inp("ab_par", [1, 32])
    G.ident_h = inp("ident_h", [128, 128], F16)
    G.ones_f = inp("ones_f", [128, 128], F32)
    G.qT_d = scr("qT_d", [GW, NT], F16)
    G.kT_d = scr("kT_d", [GW, NT], F16)
    G.ktok_d = scr("ktok_d", [NT, GW], F16)
    G.vtok_d = scr("vtok_d", [NT, GW], F16)
    G.F_d = scr("F_d", [NX, GW], F16)
    G.zs_d = scr("zs_d", [NX, GW], F16)
    G.g_d = scr("g_d", [NT, 16], F32)
    G.beta_d = scr("beta_d", [NT, 16], F32)


def phase0A(G, S, es):
    g0 = phase0_mod(G, S, es)
    for _ in range(8):
        next(g0)
    gA = phaseA_modulate(G, S, es)
    n = 0
    done0 = False
    for _ in gA:
        n += 1
        if n % 2 == 0 and not done0:
            try:
                next(g0)
            except StopIteration:
                done0 = True
    for _ in g0:
        pass


def phaseB1_qkv(G, S, es):
    nc = G.nc
    NS = 8
    def T(name, shape, dt):
        return es.enter_context(nc.sbuf_tensor(name, shape, dt))
    wq = [T("wq%d" % i, [128, KC, 512], BF16) for i in range(2)]
    hTb = [T("hB%d" % i, [128, KC, 512], BF16) for i in range(3)]
    cw = T("cw", [128, 24, 5], F32)
    diagw = T("diagw", [128, 24, 5, 128], BF16)
    identh = T("identh", [128, 128], F16)
    onesb = T("onesb", [128, 128], BF16)
    pbx = [T("pbx%d" % i, [128, 8, 68], BF16) for i in range(NS)]
    pbc = [T("pbc%d" % i, [128, 1, 260], BF16) for i in range(2)]
    sl = [T("sl%d" % i, [128, 512], F32) for i in range(NS)]
    sq = [T("sq%d" % i, [128, 512], BF16) for i in range(NS)]
    rn = [T("rn%d" % i, [128, 512], F32) for i in range(NS)]
    o16 = [T("o16_%d" % i, [128, 512], F16) for i in range(NS)]
    tk = [T("tk%d" % i, [128, 4, 128], F16) for i in range(NS)]
    S.dma("sp", lambda e: e.dma_start(out=cw[:], in_=G.conv_wT), writes=["cw"])
    S.dma("sp", lambda e: e.dma_start(out=identh[:], in_=G.ident_h), writes=["identh"])
    S.op("pool", lambda e: e.memset(onesb[:], 1.0), writes=["onesb"])
    for i in range(NS):
        S.op("pool", lambda e, i=i: e.memset(pbx[i][:], 0.0), writes=[("pbx", i)])
    for i in range(2):
        S.op("pool", lambda e, i=i: e.memset(pbc[i][:], 0.0), writes=[("pbc", i)])
    for ch in range(24):
        for j in range(5):
            S.op("dve" if (ch + j) % 2 else "pool", lambda e, ch=ch, j=j: e.tensor_scalar(out=diagw[:, ch, j, :], in0=G.ident[:], scalar1=cw[:, ch, j:j + 1], scalar2=None, op0=ALU.mult),
                 reads=["cw", "ident"], writes=[("diagw", ch)])
    wv = G.w_in.rearrange("(k p) n -> p k n", p=128)
    hv = G.hT_d.rearrange("(k p) t -> p k t", p=128)
    psh = [G.ps[6 + i][:].bitcast(F16) for i in range(2)]
    pairs = []
    nb = 0
    nctx = 0
    for grp in range(6):
        kind = grp // 2
        for bi, (t0, bw) in enumerate(TBLKS):
            if kind == 0 and bi == 0:
                continue
            hs = nb % 3
            nb += 1
            for c4 in range(4):
                pairs.append(dict(grp=grp, kind=kind, bi=bi, t0=t0, bw=bw, hs=hs, c4=c4, first=(c4 == 0), chunk=grp * 4 + c4,
                                  head=(grp * 4 + c4) % 8, wfirst=(c4 == 0 and (bi == (1 if kind == 0 else 0)))))
    for n, p in enumerate(pairs):
        p["n"] = n
        p["s"] = n % NS
        if p["bi"] == 0:
            p["cs"] = nctx % 2
            nctx += 1

    def st_proj(p):
        ws = p["grp"] % 2; hs = p["hs"]; bw = p["bw"]; t0 = p["t0"]; c4 = p["c4"]; n = p["n"]
        b = n % 2
        for k in range(KC):
            S.op("pe", lambda e, ws=ws, k=k, c4=c4, hs=hs, b=b, bw=bw: e.matmul(
                G.ps[b][:, 0:bw], lhsT=wq[ws][:, k, c4 * 128:(c4 + 1) * 128], rhs=hTb[hs][:, k, 0:bw], start=(k == 0), stop=(k == KC - 1)),
                reads=[("wq", ws), ("hB", hs)], writes=[("ps", b)])
        if p["bi"] == 0:
            cs = p["cs"]
            S.op("act", lambda e, b=b, cs=cs: e.activation(out=pbc[cs][:, :, 2:258], in_=G.ps[b][:, 0:256].rearrange("p (r l) -> p r l", l=256), func=AF.Copy),
                 reads=[("ps", b)], writes=[("pbc", cs)])
        else:
            s = p["s"]
            S.op("act", lambda e, b=b, s=s: e.activation(out=pbx[s][:, :, 2:66], in_=G.ps[b][:, :].rearrange("p (r l) -> p r l", l=64), func=AF.Copy),
                 reads=[("ps", b)], writes=[("pbx", s)])

    def st_conv(p):
        n = p["n"]; s = p["s"]; bw = p["bw"]; chunk = p["chunk"]; kind = p["kind"]
        b = 2 + n % 2
        isc = p["bi"] == 0
        L = 256 if isc else 64
        src = pbc[p["cs"]] if isc else pbx[s]
        skey = ("pbc", p["cs"]) if isc else ("pbx", s)
        for j in range(5):
            S.op("pe", lambda e, b=b, bw=bw, chunk=chunk, j=j, src=src, L=L: e.matmul(
                G.ps[b][:, 0:bw].rearrange("p (r l) -> p r l", l=L), lhsT=diagw[:, chunk, j, :], rhs=src[:, :, j:j + L], start=(j == 0), stop=(j == 4)),
                reads=[skey, ("diagw", chunk)], writes=[("ps", b)])
        if kind == 2:
            S.op("act", lambda e, s=s, b=b, bw=bw: e.activation(out=o16[s][:, 0:bw], in_=G.ps[b][:, 0:bw], func=AF.Silu),
                 reads=[("ps", b)], writes=[("o16", s)])
        else:
            S.op("act", lambda e, s=s, b=b, bw=bw: e.activation(out=sl[s][:, 0:bw], in_=G.ps[b][:, 0:bw], func=AF.Silu),
                 reads=[("ps", b)], writes=[("sl", s)])
            S.op("pool", lambda e, s=s, bw=bw: e.tensor_tensor(out=sq[s][:, 0:bw], in0=sl[s][:, 0:bw], in1=sl[s][:, 0:bw], op=ALU.mult),
                 reads=[("sl", s)], writes=[("sq", s)])

    def st_norm(p):
        if p["kind"] == 2:
            return
        n = p["n"]
        if p.get("norm_done"):
            return
        group = [p]
        if n + 1 < len(pairs) and pairs[n + 1]["kind"] != 2 and n % 2 == 0:
            return
        if n % 2 == 1 and pairs[n - 1]["kind"] != 2 and not pairs[n - 1].get("norm_done"):
            group = [pairs[n - 1], p]
        for q in group:
            s = q["s"]; bw = q["bw"]; b = 4 + q["n"] % 2
            S.op("pe", lambda e, s=s, b=b, bw=bw: e.matmul(G.ps[b][:, 0:bw], lhsT=onesb[:], rhs=sq[s][:, 0:bw], start=True, stop=True),
                 reads=[("sq", s), "onesb"], writes=[("ps", b)])
        for q in group:
            s = q["s"]; bw = q["bw"]; b = 4 + q["n"] % 2
            S.op("act", lambda e, s=s, b=b, bw=bw: e.activation(out=rn[s][:, 0:bw], in_=G.ps[b][:, 0:bw], func=AF.Sqrt, bias=EPS, scale=1.0),
                 reads=[("ps", b)], writes=[("rn", s)])
        for q in group:
            s = q["s"]; bw = q["bw"]; kind = q["kind"]; head = q["head"]; t0 = q["t0"]
            S.op("dve", lambda e, s=s, bw=bw: e.reciprocal(out=rn[s][:, 0:bw], in_=rn[s][:, 0:bw]), reads=[("rn", s)], writes=[("rn", s)])
            qs = HD ** -0.5 if kind == 0 else 1.0
            S.op("dve", lambda e, s=s, bw=bw, qs=qs: e.scalar_tensor_tensor(out=o16[s][:, 0:bw], in0=sl[s][:, 0:bw], scalar=qs, in1=rn[s][:, 0:bw], op0=ALU.mult, op1=ALU.mult),
                 reads=[("sl", s), ("rn", s)], writes=[("o16", s)])
            dstT = G.qT_d if kind == 0 else G.kT_d
            S.dma("sp", lambda e, s=s, dstT=dstT, head=head, t0=t0, bw=bw: e.dma_start(out=dstT[head * 128:(head + 1) * 128, t0:t0 + bw], in_=o16[s][:, 0:bw]),
                  reads=[("o16", s)], writes=[("qkT_d", kind, head, q["bi"])])
            q["norm_done"] = True

    def st_tr(p):
        n = p["n"]; s = p["s"]; bw = p["bw"]; kind = p["kind"]; head = p["head"]; t0 = p["t0"]
        if kind == 0:
            return
        b = n % 2
        ntl = bw // 128
        for tt in range(ntl):
            S.op("pe", lambda e, s=s, tt=tt, b=b: e.transpose(out=psh[b][:, tt * 128:(tt + 1) * 128], in_=o16[s][:, tt * 128:(tt + 1) * 128], identity=identh[:]),
                 reads=[("o16", s), "identh"], writes=[("ps", 6 + b)])
        S.op("act", lambda e, s=s, b=b, ntl=ntl: e.activation(out=tk[s][:, 0:ntl, :], in_=psh[b][:, 0:ntl * 128].rearrange("p (a b) -> p a b", b=128), func=AF.Copy),
             reads=[("ps", 6 + b)], writes=[("tk", s)])
        dst = G.ktok_d if kind == 1 else G.vtok_d
        S.dma("sp", lambda e, s=s, dst=dst, head=head, t0=t0, bw=bw, ntl=ntl: e.dma_start(
            out=dst[t0:t0 + bw, head * 128:(head + 1) * 128].rearrange("(a p) c -> p a c", p=128), in_=tk[s][:, 0:ntl, :]),
            reads=[("tk", s)], writes=[("tok_d", kind, head, p["bi"])])

    def st_load(p):
        ws = p["grp"] % 2; hs = p["hs"]; bw = p["bw"]; t0 = p["t0"]
        if p["wfirst"]:
            c0 = 1024 + p["grp"] * 512
            S.dma("pool", lambda e, ws=ws, c0=c0: e.dma_start(out=wq[ws][:], in_=wv[:, :, c0:c0 + 512]), writes=[("wq", ws)])
        if p["first"]:
            S.dma("sp", lambda e, hs=hs, t0=t0, bw=bw: e.dma_start(out=hTb[hs][:, :, 0:bw], in_=hv[:, :, t0:t0 + bw]),
                  reads=[("hT_d", p["bi"])], writes=[("hB", hs)])

    PF = 7
    for j in range(min(PF, len(pairs))):
        st_load(pairs[j])
    stages = [(0, st_proj), (1, st_conv), (3, st_norm), (6, st_tr)]
    for i in range(len(pairs) + 6):
        if i + PF < len(pairs):
            st_load(pairs[i + PF])
        for k, st in stages:
            j = i - k
            if 0 <= j < len(pairs):
                st(pairs[j])


def phaseB2_fz(G, S, es):
    nc = G.nc
    wf = es.enter_context(nc.sbuf_tensor("wf", [128, KC, 2080], BF16))
    hTb = [es.enter_context(nc.sbuf_tensor("hC%d" % i, [128, KC, 512], BF16)) for i in range(2)]
    par = es.enter_context(nc.sbuf_tensor("par", [128, 32], F32))
    ea = es.enter_context(nc.sbuf_tensor("ea", [128, 16], F32))
    o16 = [es.enter_context(nc.sbuf_tensor("fo%d" % i, [128, 2048], F16)) for i in range(2)]
    ab = [es.enter_context(nc.sbuf_tensor("ab%d" % i, [128, 32], F32)) for i in range(2)]
    gb = [es.enter_context(nc.sbuf_tensor("gb%d" % i, [128, 32], F32)) for i in range(2)]
    wv = G.w_in.rearrange("(k p) n -> p k n", p=128)
    hv = G.hT_d.rearrange("(k p) t -> p k t", p=128)
    for i in range(4):
        c0 = (0, 512, 4096, 4608)[i]
        S.dma("pool", lambda e, i=i, c0=c0: e.dma_start(out=wf[:, :, i * 512:(i + 1) * 512], in_=wv[:, :, c0:c0 + 512]), writes=[("wf", i)])
    S.dma("pool", lambda e: e.dma_start(out=wf[:, :, 2048:2080], in_=wv[:, :, 5120:5152]), writes=[("wf", 4)])
    S.dma("sp", lambda e: e.dma_start(out=par[:], in_=bc(G.ab_par[0:1, :], 32)), writes=["par"])
    S.op("act", lambda e: e.activation(out=ea[:], in_=par[:, 0:16], func=AF.Exp), reads=["par"], writes=["ea"])
    S.op("dve", lambda e: e.tensor_scalar(out=ea[:], in0=ea[:], scalar1=-1.0, scalar2=None, op0=ALU.mult), reads=["ea"], writes=["ea"])
    n = 0
    for bi, (t0, bw) in enumerate(TBLKS):
        hs = bi % 2
        S.dma("sp", lambda e, hs=hs, t0=t0, bw=bw: e.dma_start(out=hTb[hs][:, :, 0:bw], in_=hv[:, :, t0:t0 + bw]),
              reads=[("hT_d", bi)], writes=[("hC", hs)])
        for tt in range(bw // 128):
            s = n % 2
            n += 1
            tok = t0 + tt * 128
            pb = G.ps[4 + s]
            for k in range(KC):
                S.op("pe", lambda e, k=k, hs=hs, tt=tt, pb=pb: e.matmul(pb[:, 0:32], lhsT=hTb[hs][:, k, tt * 128:(tt + 1) * 128],
                                                                     rhs=wf[:, k, 2048:2080], start=(k == 0), stop=(k == KC - 1)),
                     reads=[("hC", hs), ("wf", 4)], writes=[("ps", 4 + s)])
            S.op("dve", lambda e, s=s, pb=pb: e.tensor_tensor(out=ab[s][:, 0:16], in0=pb[:, 0:16], in1=par[:, 16:32], op=ALU.add),
                 reads=[("ps", 4 + s), "par"], writes=[("ab", s)])
            S.op("act", lambda e, s=s: e.activation(out=ab[s][:, 0:16], in_=ab[s][:, 0:16], func=AF.Exp), reads=[("ab", s)], writes=[("ab", s)])
            S.op("act", lambda e, s=s: e.activation(out=ab[s][:, 0:16], in_=ab[s][:, 0:16], func=AF.Ln, bias=1.0, scale=1.0), reads=[("ab", s)], writes=[("ab", s)])
            S.op("dve", lambda e, s=s: e.tensor_tensor(out=gb[s][:, 0:16], in0=ab[s][:, 0:16], in1=ea[:], op=ALU.mult),
                 reads=[("ab", s), "ea"], writes=[("gb", s)])
            S.op("act", lambda e, s=s, pb=pb: e.activation(out=ab[s][:, 16:32], in_=pb[:, 16:32], func=AF.Exp, scale=-1.0),
                 reads=[("ps", 4 + s)], writes=[("ab2", s)])
            S.op("dve", lambda e, s=s: e.tensor_scalar(out=ab[s][:, 16:32], in0=ab[s][:, 16:32], scalar1=1.0, scalar2=None, op0=ALU.add),
                 reads=[("ab2", s)], writes=[("ab2", s)])
            S.op("dve", lambda e, s=s: e.reciprocal(out=gb[s][:, 16:32], in_=ab[s][:, 16:32]), reads=[("ab2", s)], writes=[("gb2", s)])
            S.dma("act", lambda e, s=s, tok=tok: e.dma_start(out=G.g_d[tok:tok + 128, :], in_=gb[s][:, 0:16]), reads=[("gb", s)], writes=[("g_d", tok)])
            S.dma("act", lambda e, s=s, tok=tok: e.dma_start(out=G.beta_d[tok:tok + 128, :], in_=gb[s][:, 16:32]), reads=[("gb2", s)], writes=[("beta_d", tok)])
            if bi == 0:
                continue
            for cb in range(4):
                pb2 = G.ps[cb]
                for k in range(KC):
                    S.op("pe", lambda e, k=k, hs=hs, tt=tt, pb2=pb2, cb=cb: e.matmul(
                        pb2[:, :], lhsT=hTb[hs][:, k, tt * 128:(tt + 1) * 128], rhs=wf[:, k, cb * 512:(cb + 1) * 512],
                        start=(k == 0), stop=(k == KC - 1)),
                        reads=[("hC", hs), ("wf", cb)], writes=[("ps", cb)])
                if cb < 2:
                    S.op("dve", lambda e, s=s, cb=cb, pb2=pb2: e.tensor_copy(out=o16[s][:, cb * 512:(cb + 1) * 512], in_=pb2[:, :]),
                         reads=[("ps", cb)], writes=[("fo", s, cb)])
                else:
                    S.op("act", lambda e, s=s, cb=cb, pb2=pb2: e.activation(out=o16[s][:, cb * 512:(cb + 1) * 512], in_=pb2[:, :], func=AF.Silu),
                         reads=[("ps", cb)], writes=[("fo", s, cb)])
            xt0 = tok - NCTX
            S.dma("act", lambda e, s=s, xt0=xt0: e.dma_start(out=G.F_d[xt0:xt0 + 128, :], in_=o16[s][:, 0:1024]),
                  reads=[("fo", s, 0), ("fo", s, 1)], writes=[("F_d", xt0)])
            S.dma("act", lambda e, s=s, xt0=xt0: e.dma_start(out=G.zs_d[xt0:xt0 + 128, :], in_=o16[s][:, 1024:2048]),
                  reads=[("fo", s, 2), ("fo", s, 3)], writes=[("zs_d", xt0)])


def declareG(G, nc, dbg=()):
    def inp(name, shape, dt=F32):
        return nc.dram_tensor(name, list(shape), dt, kind="ExternalInput").ap()
    def scr(name, shape, dt):
        kind = "ExternalOutput" if name in dbg else "Internal"
        return nc.dram_tensor(name, list(shape), dt, kind=kind).ap()
    G.cmask = inp("cmask", [128, 2, 6, 128])
    G.o_d = [scr("o_d%d" % d, [NX, GW], F32) for d in range(2)]


def gdn_masks():
    i = np.arange(128)[:, None]; j = np.arange(128)[None, :]
    m = np.zeros((128, 2, 6, 128), np.float32)
    bi, bj = i // 32, j // 32
    m[:, 0, 0] = (i <= j)
    m[:, 1, 0] = (i >= j)
    m[:, 0, 1] = (i > j)
    m[:, 1, 1] = (i < j)
    m[:, 0, 2] = (i > j) & (bi == bj)
    m[:, 1, 2] = (i < j) & (bi == bj)
    m[:, 0, 3] = (bi % 2 == 1) & (bj == bi - 1)
    m[:, 1, 3] = (bi % 2 == 0) & (bj == bi + 1)
    m[:, 0, 4] = (i >= 64) & (j < 64)
    m[:, 1, 4] = (i < 64) & (j >= 64)
    m[:, 0, 5] = (j >= i)
    m[:, 1, 5] = (j <= i)
    return m


def phaseG_gdn(G, S, es):
    nc = G.nc
    import os
    def T(name, shape, dt):
        return es.enter_context(nc.sbuf_tensor(name, shape, dt))
    cm = T("cm", [128, 2, 6, 128], F32)
    identh = T("identh2", [128, 128], F16)
    onesf = T("onesf2", [128, 128], F32)
    eye32 = T("eye32", [128, 128], F32)
    S.dma("sp", lambda e: e.dma_start(out=cm[:], in_=G.cmask), writes=["cm"])
    S.dma("sp", lambda e: e.dma_start(out=identh[:], in_=G.ident_h), writes=["identh2"])
    S.dma("sp", lambda e: e.dma_start(out=onesf[:], in_=G.ones_f), writes=["onesf2"])
    S.op("act", lambda e: e.activation(out=eye32[:], in_=identh[:], func=AF.Copy), reads=["identh2"], writes=["eye32"])
    psh = [G.ps[b][:].bitcast(F16) for b in range(8)]
    state = {"bank": 0}

    def banks():
        b = state["bank"]
        state["bank"] = (b + 1) % 8
        return b, b

    def bch(ap2):
        return bass.AP(ap2.tensor, ap2.offset, [list(ap2.ap[0]), [0, NHS], list(ap2.ap[1])])

    def bcj(ap2, n=NH):
        return bass.AP(ap2.tensor, ap2.offset, [list(ap2.ap[0]), list(ap2.ap[1]), [0, 128]])

    def v32(b):
        return G.ps[b][:, :].rearrange("p (q n) -> p q n", q=4)

    def v16(b):
        return psh[b][:, :].rearrange("p (q n) -> p q n", q=4)[:, :, 0:128]

    NHS = 4
    def make_dir(d, hg):
        X = "d%d_%d_" % (d, hg)
        def H(name, dt):
            return T(X + name, [128, NHS, 128], dt)
        kT2 = [T(X + "kT%d" % i, [128, NHS, 128], F16) for i in range(2)]; qT2 = [T(X + "qT%d" % i, [128, NHS, 128], F16) for i in range(2)]
        kt2 = [T(X + "kt%d" % i, [128, NHS, 128], F16) for i in range(2)]; vt2 = [T(X + "vt%d" % i, [128, NHS, 128], F16) for i in range(2)]
        gu2 = [T(X + "gu%d" % i, [128, 16], F32) for i in range(2)]; bu2 = [T(X + "bu%d" % i, [128, 16], F32) for i in range(2)]
        decb = H("decb", F32)
        egc = T(X + "egc", [128, NHS], F32); erem = T(X + "erem", [128, NHS], F32); etot = T(X + "etot", [128, NHS], F32)
        gcs = T(X + "gcs", [128, NHS], F32); bsc = T(X + "bsc", [128, NHS], F32)
        S32 = H("S32", F32); S16 = H("S16", F16)
        G2 = H("G2", F32); dec = H("dec", F32); decT = H("decT", F32)
        P = [H("Pa", F16), H("Pb", F16)]; PT = [H("PTa", F16), H("PTb", F16)]
        Mo1 = H("Mo1", F16); Mo1T = H("Mo1T", F16); Mo2 = H("Mo2", F16)
        Rr = [H("Ra", F16), H("Rb", F16)]; Tt = [H("Ta", F16), H("Tb", F16)]
        Z = H("Z", F16); Z2 = H("Z2", F16)
        qkm = H("qkm", F16); kb = H("kb", F16); vb = H("vb", F16); kdc = H("kdc", F16)
        uu = H("uu", F32); wT = H("wT", F16); vnew = H("vnew", F16); otmp = H("otmp", F32); oo = H("oo", F32)
        K0 = lambda n: (X + n)
        K = K0
        ALLG = (0, 1)

        def smm(fl, fr, rd):
            b0, b1 = banks()
            for h in range(NHS):
                b = b0
                q = h
                l_ = fl(h); r_ = fr(h)
                S.op("pe", lambda e, b=b, q=q, l_=l_, r_=r_: e.matmul(G.ps[b][:, q * 128:(q + 1) * 128], lhsT=l_, rhs=r_, start=True, stop=True),
                     reads=rd, writes=[("ps", b)])
            return (b0,)

        def strp(src, rd):
            b0, b1 = banks()
            for h in range(NHS):
                b = b0
                q = h
                S.op("pe", lambda e, b=b, q=q, h=h: e.transpose(out=psh[b][:, q * 256:q * 256 + 128], in_=src[:, h, :], identity=identh[:]),
                     reads=rd + ["identh2"], writes=[("ps", b)])
            return (b0,)

        def evac(eng, bb, dst, dkey, f16=False, fn=None, extra=()):
            for g, b in enumerate(bb):
                src = v16(b) if f16 else v32(b)
                o = dst[:, 4 * g:4 * g + 4, :]
                if fn is None:
                    if eng == "act":
                        S.op("act", lambda e, o=o, src=src: e.activation(out=o, in_=src, func=AF.Copy), reads=[("ps", b)] + list(extra), writes=[dkey])
                    else:
                        S.op("dve", lambda e, o=o, src=src: e.tensor_copy(out=o, in_=src), reads=[("ps", b)] + list(extra), writes=[dkey])
                else:
                    S.op(eng, (lambda e, o=o, src=src, g=g: fn(e, o, src, g)), reads=[("ps", b)] + list(extra), writes=[dkey])

        S.op("pool", lambda e: e.memset(S32[:], 0.0), writes=[K("S32")])
        S.op("pool", lambda e: e.memset(S16[:], 0.0), writes=[K("S16")])
        nunits = NT // 128
        order = list(range(nunits)) if d == 0 else [1, 0] + list(range(nunits - 1, 1, -1))
        order = order[:int(os.environ.get("GUNITS", "99"))]
        c8 = slice(d * 8 + hg * 4, d * 8 + hg * 4 + 4)
        hr = slice(hg * 512, (hg + 1) * 512)
        A1 = cm[:, d, 0, :]; B1 = cm[:, d, 1, :]

        def loads(u, sl_):
            tok = u * 128
            kT, qT, kt, vt, gu, bu = kT2[sl_], qT2[sl_], kt2[sl_], vt2[sl_], gu2[sl_], bu2[sl_]
            ks = str(sl_)
            S.dma("sp", lambda e: e.dma_start(out=kT[:], in_=G.kT_d[hr, tok:tok + 128].rearrange("(h p) t -> p h t", p=128)),
                  reads=[("qkT_d", 1, h, b_) for h in range(NH) for b_ in range(9)], writes=[K("kT" + ks)])
            if u >= 2:
                S.dma("sp", lambda e: e.dma_start(out=qT[:], in_=G.qT_d[hr, tok:tok + 128].rearrange("(h p) t -> p h t", p=128)),
                      reads=[("qkT_d", 0, h, b_) for h in range(NH) for b_ in range(1, 9)], writes=[K("qT" + ks)])
            S.dma("sp", lambda e: e.dma_start(out=kt[:].rearrange("p h c -> p (h c)"), in_=G.ktok_d[tok:tok + 128, hr]),
                  reads=[("tok_d", 1, h, b_) for h in range(NH) for b_ in range(9)], writes=[K("kt" + ks)])
            S.dma("sp", lambda e: e.dma_start(out=vt[:].rearrange("p h c -> p (h c)"), in_=G.vtok_d[tok:tok + 128, hr]),
                  reads=[("tok_d", 2, h, b_) for h in range(NH) for b_ in range(9)], writes=[K("vt" + ks)])
            S.dma("sp", lambda e: e.dma_start(out=gu[:], in_=G.g_d[tok:tok + 128, :]), reads=[("g_d", tok)], writes=[K("gu" + ks)])
            S.dma("sp", lambda e: e.dma_start(out=bu[:], in_=G.beta_d[tok:tok + 128, :]), reads=[("beta_d", tok)], writes=[K("bu" + ks)])

        def unit(u, ui, unext):
            tok = u * 128
            isx = u >= 2
            sl_ = ui % 2
            kT, qT, kt, vt, gu, bu = kT2[sl_], qT2[sl_], kt2[sl_], vt2[sl_], gu2[sl_], bu2[sl_]
            def K(n, sl_=sl_):
                return K0(n + str(sl_)) if n in ("kT", "qT", "kt", "vt", "gu", "bu") else K0(n)
            if ui == 0:
                loads(u, 0)
            if unext is not None:
                loads(unext, 1 - sl_)
            b0, _ = banks()
            S.op("pe", lambda e: e.matmul(G.ps[b0][:, 0:4], lhsT=A1, rhs=gu[:, c8], start=True, stop=True), reads=["cm", K("gu")], writes=[("ps", b0)])
            S.op("pe", lambda e: e.matmul(G.ps[b0][:, 8:12], lhsT=onesf[:], rhs=gu[:, c8], start=True, stop=True), reads=["onesf2", K("gu")], writes=[("ps", b0)])
            S.op("act", lambda e: e.activation(out=egc[:], in_=G.ps[b0][:, 0:4], func=AF.Exp), reads=[("ps", b0)], writes=[K("egc")])
            S.op("act", lambda e: e.activation(out=etot[:], in_=G.ps[b0][:, 8:12], func=AF.Exp), reads=[("ps", b0)], writes=[K("etot")])
            S.op("act", lambda e: e.activation(out=gcs[:], in_=G.ps[b0][:, 0:4], func=AF.Copy), reads=[("ps", b0)], writes=[K("gcs")])
            S.op("dve", lambda e: e.tensor_tensor(out=erem[:], in0=G.ps[b0][:, 8:12], in1=gcs[:], op=ALU.subtract), reads=[("ps", b0), K("gcs")], writes=[K("erem")])
            S.op("act", lambda e: e.activation(out=erem[:], in_=erem[:], func=AF.Exp), reads=[K("erem")], writes=[K("erem")])
            S.op("dve", lambda e: e.tensor_tensor(out=bsc[:], in0=bu[:, c8], in1=egc[:], op=ALU.mult), reads=[K("bu"), K("egc")], writes=[K("bsc")])
            S.op("pool", lambda e: e.tensor_tensor(out=G2[:], in0=bch(B1), in1=bcj(gu[:, c8]), op=ALU.mult), reads=["cm", K("gu")], writes=[K("G2")])
            yield
            bb = smm(lambda h: A1, lambda h: G2[:, h, :], ["cm", K("G2")])
            evac("act", bb, dec, K("dec"), fn=lambda e, o, src, g: e.activation(out=o, in_=src, func=AF.Exp))
            S.op("pool", lambda e: e.tensor_tensor(out=dec[:], in0=dec[:], in1=bcj(bu[:, c8]), op=ALU.mult), reads=[K("dec"), K("bu")], writes=[K("dec")])
            S.op("pool", lambda e: e.tensor_tensor(out=decb[:], in0=dec[:], in1=bch(cm[:, d, 2, :]), op=ALU.mult), reads=[K("dec"), "cm"], writes=[K("decb")])
            yield
            bb = smm(lambda h: G2[:, h, :], lambda h: A1, ["cm", K("G2")])
            evac("act", bb, decT, K("decT"), fn=lambda e, o, src, g: e.activation(out=o, in_=src, func=AF.Exp))
            S.op("pool", lambda e: e.tensor_tensor(out=decT[:], in0=decT[:], in1=bch(cm[:, d, 5, :]), op=ALU.mult), reads=[K("decT"), "cm"], writes=[K("decT")])
            yield
            bb = smm(lambda h: kT[:, h, :], lambda h: kT[:, h, :], [K("kT")])
            evac("dve", bb, P[0], K("P0"), fn=lambda e, o, src, g: e.tensor_tensor(out=o, in0=src, in1=decb[:], op=ALU.mult), extra=[K("decb")])
            evac("dve", bb, G2, K("G2"), fn=lambda e, o, src, g: e.tensor_tensor(out=o, in0=src, in1=dec[:], op=ALU.mult), extra=[K("dec")])
            S.op("pool", lambda e: e.tensor_tensor(out=Mo1[:], in0=G2[:], in1=bch(cm[:, d, 3, :]), op=ALU.mult), reads=[K("G2"), "cm"], writes=[K("Mo1")])
            S.op("pool", lambda e: e.tensor_tensor(out=Mo2[:], in0=G2[:], in1=bch(cm[:, d, 4, :]), op=ALU.mult), reads=[K("G2"), "cm"], writes=[K("Mo2")])
            S.op("dve", lambda e: e.tensor_tensor(out=kb[:], in0=kt[:], in1=bcj(bsc[:]), op=ALU.mult), reads=[K("kt"), K("bsc")], writes=[K("kb")])
            S.op("dve", lambda e: e.tensor_tensor(out=vb[:], in0=vt[:], in1=bcj(bu[:, c8]), op=ALU.mult), reads=[K("vt"), K("bu")], writes=[K("vb")])
            S.op("pool", lambda e: e.tensor_tensor(out=kdc[:], in0=kt[:], in1=bcj(erem[:]), op=ALU.mult), reads=[K("kt"), K("erem")], writes=[K("kdc")])
            yield
            if isx:
                bb = smm(lambda h: kT[:, h, :], lambda h: qT[:, h, :], [K("kT"), K("qT")])
                evac("dve", bb, qkm, K("qkm"), fn=lambda e, o, src, g: e.tensor_tensor(out=o, in0=src, in1=decT[:, 4 * g:4 * g + 4, :], op=ALU.mult), extra=[K("decT")])
                yield
            bb = strp(P[0], [K("P0")])
            evac("act", bb, PT[0], K("PT0"), f16=True)
            yield
            bb = strp(Mo1, [K("Mo1")])
            evac("act", bb, Mo1T, K("Mo1T"), f16=True)
            S.op("pool", lambda e: e.tensor_tensor(out=Rr[0][:], in0=bch(eye32[:]), in1=PT[0][:], op=ALU.subtract), reads=["eye32", K("PT0")], writes=[K("R0")])
            S.op("pool", lambda e: e.tensor_tensor(out=Tt[0][:], in0=bch(eye32[:]), in1=P[0][:], op=ALU.subtract), reads=["eye32", K("P0")], writes=[K("T0")])
            yield
            cur = 0
            for lvl in range(4):
                nxt = 1 - cur
                Pc, PTc, Pn, PTn, Rc, Rn, Tc, Tn = P[cur], PT[cur], P[nxt], PT[nxt], Rr[cur], Rr[nxt], Tt[cur], Tt[nxt]
                kc_, kn_ = str(cur), str(nxt)
                bb = smm(lambda h: PTc[:, h, :], lambda h: Pc[:, h, :], [K("PT" + kc_), K("P" + kc_)])
                evac("act", bb, Pn, K("P" + kn_))
                yield
                if lvl < 3:
                    bb = smm(lambda h: Pc[:, h, :], lambda h: PTc[:, h, :], [K("PT" + kc_), K("P" + kc_)])
                    evac("act", bb, PTn, K("PT" + kn_))
                    yield
                bb = smm(lambda h: Pn[:, h, :], lambda h: Rc[:, h, :], [K("P" + kn_), K("R" + kc_)])
                evac("dve", bb, Rn, K("R" + kn_), fn=lambda e, o, src, g, Rc=Rc: e.tensor_tensor(out=o, in0=Rc[:, 4 * g:4 * g + 4, :], in1=src, op=ALU.add), extra=[K("R" + kc_)])
                yield
                bb = smm(lambda h: Rc[:, h, :], lambda h: Pn[:, h, :], [K("P" + kn_), K("R" + kc_)])
                evac("dve", bb, Tn, K("T" + kn_), fn=lambda e, o, src, g, Tc=Tc: e.tensor_tensor(out=o, in0=Tc[:, 4 * g:4 * g + 4, :], in1=src, op=ALU.add), extra=[K("T" + kc_)])
                yield
                cur = nxt
            nxt = 1 - cur
            Rc, Rn, Tc, Tn = Rr[cur], Rr[nxt], Tt[cur], Tt[nxt]
            kc_, kn_ = str(cur), str(nxt)
            bb = smm(lambda h: Mo1[:, h, :], lambda h: Rc[:, h, :], [K("Mo1"), K("R" + kc_)])
            evac("act", bb, Z, K("Z"))
            yield
            bb = smm(lambda h: Mo1T[:, h, :], lambda h: Tc[:, h, :], [K("Mo1T"), K("T" + kc_)])
            evac("act", bb, Z2, K("Z2"))
            yield
            bb = smm(lambda h: Tc[:, h, :], lambda h: Z[:, h, :], [K("T" + kc_), K("Z")])
            evac("dve", bb, Rn, K("R" + kn_), fn=lambda e, o, src, g, Rc=Rc: e.tensor_tensor(out=o, in0=Rc[:, 4 * g:4 * g + 4, :], in1=src, op=ALU.subtract), extra=[K("R" + kc_)])
            yield
            bb = smm(lambda h: Rc[:, h, :], lambda h: Z2[:, h, :], [K("R" + kc_), K("Z2")])
            evac("dve", bb, Tn, K("T" + kn_), fn=lambda e, o, src, g, Tc=Tc: e.tensor_tensor(out=o, in0=Tc[:, 4 * g:4 * g + 4, :], in1=src, op=ALU.subtract), extra=[K("T" + kc_)])
            yield
            cur = nxt
            nxt = 1 - cur
            Rc, Rn, Tc = Rr[cur], Rr[nxt], Tt[cur]
            kc_, kn_ = str(cur), str(nxt)
            bb = smm(lambda h: Mo2[:, h, :], lambda h: Rc[:, h, :], [K("Mo2"), K("R" + kc_)])
            evac("act", bb, Z, K("Z"))
            yield
            bb = smm(lambda h: Tc[:, h, :], lambda h: Z[:, h, :], [K("T" + kc_), K("Z")])
            evac("dve", bb, Rn, K("R" + kn_), fn=lambda e, o, src, g, Rc=Rc: e.tensor_tensor(out=o, in0=Rc[:, 4 * g:4 * g + 4, :], in1=src, op=ALU.subtract), extra=[K("R" + kc_)])
            yield
            Rf = Rn; rk = K("R" + kn_)
            bb = smm(lambda h: Rf[:, h, :], lambda h: vb[:, h, :], [rk, K("vb")])
            evac("act", bb, uu, K("uu"))
            yield
            bb = smm(lambda h: kb[:, h, :], lambda h: Rf[:, h, :], [rk, K("kb")])
            evac("act", bb, wT, K("wT"))
            yield
            bb = smm(lambda h: wT[:, h, :], lambda h: S16[:, h, :], [K("wT"), K("S16")])
            evac("dve", bb, vnew, K("vnew"), fn=lambda e, o, src, g: e.tensor_tensor(out=o, in0=uu[:, 4 * g:4 * g + 4, :], in1=src, op=ALU.subtract), extra=[K("uu")])
            yield
            if isx:
                bb = smm(lambda h: qT[:, h, :], lambda h: S16[:, h, :], [K("qT"), K("S16")])
                evac("dve", bb, otmp, K("otmp"), fn=lambda e, o, src, g: e.tensor_tensor(out=o, in0=src, in1=bcj(egc[:, 4 * g:4 * g + 4]), op=ALU.mult), extra=[K("egc")])
                yield
                bb = smm(lambda h: qkm[:, h, :], lambda h: vnew[:, h, :], [K("qkm"), K("vnew")])
                evac("dve", bb, oo, K("oo"), fn=lambda e, o, src, g: e.tensor_tensor(out=o, in0=otmp[:, 4 * g:4 * g + 4, :], in1=src, op=ALU.add), extra=[K("otmp")])
                xt0 = tok - NCTX
                S.dma("sp", lambda e: e.dma_start(out=G.o_d[d][xt0:xt0 + 128, hr], in_=oo[:].rearrange("p h v -> p (h v)")),
                      reads=[K("oo")], writes=[("o_d", d, xt0, hg)])
                yield
            bb = smm(lambda h: kdc[:, h, :], lambda h: vnew[:, h, :], [K("kdc"), K("vnew")])
            S.op("pool", lambda e: e.tensor_tensor(out=S32[:], in0=S32[:], in1=bcj(etot[:]), op=ALU.mult), reads=[K("S32"), K("etot")], writes=[K("S32")])
            evac("dve", bb, S32, K("S32"), fn=lambda e, o, src, g: e.tensor_tensor(out=o, in0=S32[:, 4 * g:4 * g + 4, :], in1=src, op=ALU.add), extra=[K("S32")])
            S.op("act", lambda e: e.activation(out=S16[:], in_=S32[:], func=AF.Copy), reads=[K("S32")], writes=[K("S16")])
            yield

        def run():
            for ui, u in enumerate(order):
                yield from unit(u, ui, order[ui + 1] if ui + 1 < len(order) else None)
        return run()

    gens = [make_dir(0, 0), make_dir(1, 0), make_dir(0, 1), make_dir(1, 1)]
    alive = [True] * 4
    while any(alive):
        for i, g in enumerate(gens):
            if alive[i]:
                try:
                    next(g)
                except StopIteration:
                    alive[i] = False


def declareF(G, nc, dbg=()):
    def inp(name, shape, dt=F32):
        return nc.dram_tensor(name, list(shape), dt, kind="ExternalInput").ap()
    def scr(name, shape, dt):
        kind = "ExternalOutput" if name in dbg else "Internal"
        return nc.dram_tensor(name, list(shape), dt, kind=kind).ap()
    G.W1 = inp("W1", [64, 128], F16)
    G.W2 = inp("W2", [128, 2, 256], F16)
    G.W3 = inp("W3", [64, 64, 2, 64], F16)
    G.gnw = inp("gnw", [1, 128])
    G.mixT_d = scr("mixT_d", [D, NX], BF16)


def fourier_tables():
    a = np.arange(64)
    ang = 2 * np.pi * np.outer(a, a) / 64
    W1 = np.concatenate([np.cos(ang), -np.sin(ang)], 1)
    c = np.arange(128)
    ang2 = 2 * np.pi * np.outer(c, c) / 128
    W2 = np.stack([np.concatenate([np.cos(ang2), -np.sin(ang2)], 1),
                   np.concatenate([np.sin(ang2), np.cos(ang2)], 1)], 1)
    b = np.arange(64)[:, None, None]; o2 = np.arange(64)[None, :, None]; o1 = np.arange(64)[None, None, :]
    th = 2 * np.pi * (b * o2 / 4096.0 + b * o1 / 64.0)
    sc = 1.0 / np.sqrt(4096 * 128)
    W3 = np.stack([np.cos(th) * sc, np.sin(th) * sc], 2)
    return W1.astype(np.float16), W2.astype(np.float16), W3.astype(np.float16)


def phaseF_fourier(G, S, es):
    nc = G.nc
    def T(name, shape, dt):
        return es.enter_context(nc.sbuf_tensor(name, shape, dt))
    w1 = T("w1", [64, 128], F16); w2 = T("w2", [128, 2, 256], F16); w3 = T("w3", [64, 64, 2, 64], F16)
    S.dma("sp", lambda e: e.dma_start(out=w1[:], in_=G.W1), writes=["w1"])
    S.dma("sp", lambda e: e.dma_start(out=w2[:], in_=G.W2), writes=["w2"])
    S.dma("sp", lambda e: e.dma_start(out=w3[:], in_=G.W3), writes=["w3"])
    Fg = [T("Fg%d" % i, [64, 64, 256], F16) for i in range(2)]
    P1 = T("P1", [128, 64, 128], F16)
    J = T("J", [64, 64, 256], F16)
    YT = [T("YT%d" % i, [128, NX], BF16) for i in range(2)]
    Fv = G.F_d.rearrange("(a b) c -> a b c", b=64)
    nb = 0
    def nextbank():
        nonlocal nb
        b = nb % 8
        nb += 1
        return b
    ev = 0
    for gp in range(4):
        fs = gp % 2
        S.dma("sp", lambda e, fs=fs, gp=gp: e.dma_start(out=Fg[fs][:], in_=Fv[:, :, gp * 256:(gp + 1) * 256]),
              reads=[("F_d", t * 128) for t in range(32)], writes=[("Fg", fs)])
        for gi in range(2):
            g = gp * 2 + gi
            ys = g % 2
            for b4 in range(16):
                bk = nextbank()
                for q in range(4):
                    b = b4 * 4 + q
                    S.op("pe", lambda e, fs=fs, gi=gi, b=b, bk=bk, q=q: e.matmul(G.ps[bk][:, q * 128:(q + 1) * 128],
                         lhsT=Fg[fs][:, b, gi * 128:(gi + 1) * 128], rhs=w1[:], start=True, stop=True),
                         reads=[("Fg", fs), "w1"], writes=[("psb", bk)])
                eng = "act" if ev % 2 == 0 else "dve"; ev += 1
                def f(e, bk=bk, b4=b4, eng=eng):
                    o = P1[:, b4 * 4:(b4 + 1) * 4, :]
                    i = G.ps[bk][:, :].rearrange("p (q n) -> p q n", q=4)
                    return e.activation(out=o, in_=i, func=AF.Copy) if eng == "act" else e.tensor_copy(out=o, in_=i)
                S.op(eng, f, reads=[("psb", bk)], writes=[("P1", b4)])
            for o22 in range(32):
                bk = nextbank()
                for q in range(2):
                    o2 = o22 * 2 + q
                    for ri in range(2):
                        S.op("pe", lambda e, o2=o2, ri=ri, bk=bk, q=q: e.matmul(G.ps[bk][0:64, q * 256:(q + 1) * 256],
                             lhsT=P1[:, :, ri * 64 + o2], rhs=w2[:, ri, :], start=(ri == 0), stop=(ri == 1)),
                             reads=[("P1", x_) for x_ in range(16)] + ["w2"], writes=[("psb", bk)])
                eng = "act" if ev % 2 == 0 else "dve"; ev += 1
                def f(e, bk=bk, o22=o22, eng=eng):
                    o = J[:, o22 * 2:(o22 + 1) * 2, :]
                    i = G.ps[bk][0:64, :].rearrange("p (q n) -> p q n", q=2)
                    return e.activation(out=o, in_=i, func=AF.Copy) if eng == "act" else e.tensor_copy(out=o, in_=i)
                S.op(eng, f, reads=[("psb", bk)], writes=[("J", o22)])
            for o28 in range(8):
                bk = nextbank()
                for q in range(8):
                    o2 = o28 * 8 + q
                    for ri in range(2):
                        S.op("pe", lambda e, o2=o2, ri=ri, bk=bk, q=q: e.matmul(G.ps[bk][:, q * 64:(q + 1) * 64],
                             lhsT=J[:, o2, ri * 128:(ri + 1) * 128], rhs=w3[:, o2, ri, :], start=(ri == 0), stop=(ri == 1)),
                             reads=[("J", x_) for x_ in range(32)] + ["w3"], writes=[("psb", bk)])
                eng = "act" if ev % 2 == 0 else "dve"; ev += 1
                def f(e, bk=bk, o28=o28, eng=eng, ys=ys):
                    o = YT[ys][:, :].rearrange("p (o1 o2) -> p o2 o1", o2=64)[:, o28 * 8:(o28 + 1) * 8, :]
                    i = G.ps[bk][:, :].rearrange("p (q n) -> p q n", q=8)
                    return e.activation(out=o, in_=i, func=AF.Copy) if eng == "act" else e.tensor_copy(out=o, in_=i)
                S.op(eng, f, reads=[("psb", bk)], writes=[("YT", ys, o28)])
            S.dma("act", lambda e, ys=ys, g=g: e.dma_start(out=G.mixT_d[g * 128:(g + 1) * 128, :], in_=YT[ys][:]),
                  reads=[("YT", ys, x_) for x_ in range(8)], writes=[("mixT_d", g)])


def phaseO_gdnout(G, S, es):
    nc = G.nc
    def T(name, shape, dt):
        return es.enter_context(nc.sbuf_tensor(name, shape, dt))
    gn = T("gn", [128, 128], F32)
    S.dma("sp", lambda e: e.dma_start(out=gn[:], in_=bc(G.gnw[0:1, :], 128)), writes=["gn"])
    of = [T("of%d" % i, [128, GW], F32) for i in range(4)]
    ob = [T("ob%d" % i, [128, GW], F32) for i in range(4)]
    zt = [T("zt%d" % i, [128, GW], F16) for i in range(4)]
    sqt = T("sqt", [128, GW], F32)
    ms = [T("ms%d" % i, [128, NH], F32) for i in range(2)]
    on = [T("on%d" % i, [128, GW], BF16) for i in range(2)]
    oT = [T("oT%d" % i, [128, NH, 128], BF16) for i in range(2)]
    ident = G.ident

    def o_load(t):
        s4 = t % 4; r0 = t * 128
        S.dma("sp", lambda e: e.dma_start(out=of[s4][:], in_=G.o_d[0][r0:r0 + 128, :]), reads=[("o_d", 0, r0, 0), ("o_d", 0, r0, 1)], writes=[("of", s4)])
        S.dma("sp", lambda e: e.dma_start(out=ob[s4][:], in_=G.o_d[1][r0:r0 + 128, :]), reads=[("o_d", 1, r0, 0), ("o_d", 1, r0, 1)], writes=[("ob", s4)])
        S.dma("sp", lambda e: e.dma_start(out=zt[s4][:], in_=G.zs_d[r0:r0 + 128, :]), reads=[("zs_d", r0)], writes=[("zt", s4)])

    def o_stat(t):
        s4 = t % 4; s = t % 2
        S.op("dve", lambda e: e.tensor_tensor(out=of[s4][:], in0=of[s4][:], in1=ob[s4][:], op=ALU.add), reads=[("of", s4), ("ob", s4)], writes=[("of", s4)])
        S.op("pool", lambda e: e.tensor_tensor(out=sqt[:], in0=of[s4][:], in1=of[s4][:], op=ALU.mult), reads=[("of", s4)], writes=["sqt"])
        S.op("dve", lambda e: e.tensor_reduce(out=ms[s][:], in_=sqt[:].rearrange("p (h v) -> p h v", v=128), axis=AX.X, op=ALU.add),
             reads=["sqt"], writes=[("ms", s)])
        S.op("act", lambda e: e.activation(out=ms[s][:], in_=ms[s][:], func=AF.Sqrt, bias=EPS, scale=1.0 / 128), reads=[("ms", s)], writes=[("ms", s)])
        S.op("dve", lambda e: e.reciprocal(out=ms[s][:], in_=ms[s][:]), reads=[("ms", s)], writes=[("ms", s)])

    def o_scale(t):
        s4 = t % 4; s = t % 2
        for h in range(NH):
            hs_ = slice(h * 128, (h + 1) * 128)
            S.op("dve", lambda e, h=h, hs_=hs_: e.scalar_tensor_tensor(out=of[s4][:, hs_], in0=of[s4][:, hs_], scalar=ms[s][:, h:h + 1], in1=gn[:], op0=ALU.mult, op1=ALU.mult),
                 reads=[("of", s4), ("ms", s), "gn"], writes=[("of", s4)])
        S.op("pool", lambda e: e.tensor_tensor(out=on[s][:], in0=of[s4][:], in1=zt[s4][:], op=ALU.mult), reads=[("of", s4), ("zt", s4)], writes=[("on", s)])

    def o_tr(t):
        s = t % 2; r0 = t * 128
        bk = 6 + s
        for h in range(NH):
            S.op("pe", lambda e, h=h: e.transpose(out=G.pst[s][:, h * 128:(h + 1) * 128], in_=on[s][:, h * 128:(h + 1) * 128], identity=ident[:]),
                 reads=[("on", s), "ident"], writes=[("ps", bk)])
        S.op("act", lambda e: e.activation(out=oT[s][:], in_=G.pst[s][:, :].rearrange("p (h t) -> p h t", h=8), func=AF.Copy),
             reads=[("ps", bk)], writes=[("oT", s)])
        S.dma("sp", lambda e: e.dma_start(out=G.mixT_d[GW:2 * GW, r0:r0 + 128].rearrange("(h p) t -> p h t", p=128), in_=oT[s][:]),
              reads=[("oT", s)], writes=[("mixT_d", 8 + t)])

    stages = [(0, o_load), (2, o_stat), (3, o_scale), (4, o_tr)]
    for i in range(32 + 4):
        for k_, st in stages:
            j = i - k_
            if 0 <= j < 32:
                st(j)


NE = 16
CAP = 512
FF = 1536


def declareW(G, nc, dbg=()):
    def inp(name, shape, dt=F32):
        return nc.dram_tensor(name, list(shape), dt, kind="ExternalInput").ap()
    def scr(name, shape, dt):
        kind = "ExternalOutput" if name in dbg else "Internal"
        return nc.dram_tensor(name, list(shape), dt, kind=kind).ap()
    G.w_out = inp("w_out", [D, D])
    G.w_router = inp("w_router", [D, NE])
    G.ident_f = inp("ident_f", [128, 128], F32)
    G.w_gate = inp("w_gate", [NE, D, FF])
    G.w_up = inp("w_up", [NE, D, FF])
    G.w_down = inp("w_down", [NE, FF, D])
    G.norm_f = inp("norm_f", [1, D])
    G.x1_d = scr("x1_d", [NX, D], F32)
    G.hx2_d = scr("hx2_d", [NX, D], BF16)
    G.affT_d = scr("affT_d", [NE, NX], F32)
    G.cand_d = scr("cand_d", [128, 104], F32)


def phaseW_out(G, S, es):
    nc = G.nc
    def T(name, shape, dt):
        return es.enter_context(nc.sbuf_tensor(name, shape, dt))
    wo = T("wo", [128, KC, D], BF16)
    wr = T("wr", [128, KC, NE], BF16)
    identf = T("identf", [128, 128], F32)
    gt1 = T("gt1", [128, D], F32); gm2 = T("gm2", [128, D], F32); sh2 = T("sh2", [128, D], F32)
    wov = G.w_out.rearrange("(k p) n -> p k n", p=128)
    for i in range(4):
        S.dma("pool", lambda e, i=i: e.dma_start(out=wo[:, :, i * 512:(i + 1) * 512], in_=wov[:, :, i * 512:(i + 1) * 512]), writes=[("wo", i)])
    S.dma("pool", lambda e: e.dma_start(out=wr[:], in_=G.w_router.rearrange("(k p) n -> p k n", p=128)), writes=["wr"])
    S.dma("sp", lambda e: e.dma_start(out=identf[:], in_=G.ident_f), writes=["identf"])
    S.dma("sp", lambda e: e.dma_start(out=gt1[:], in_=bc(G.mod_d[0:1, 2 * D:3 * D], D)), reads=["mod_d"], writes=["gt1"])
    S.dma("sp", lambda e: e.dma_start(out=sh2[:], in_=bc(G.mod_d[0:1, 3 * D:4 * D], D)), reads=["mod_d"], writes=["sh2"])
    S.dma("sp", lambda e: e.dma_start(out=gm2[:], in_=bc(G.mod_d[0:1, 4 * D:5 * D], D)), reads=["mod_d"], writes=["gm2"])
    mT = [T("mT%d" % i, [128, KC, 128], BF16) for i in range(2)]
    xt = [T("wxt%d" % i, [128, D], F32) for i in range(2)]
    x1 = [T("x1_%d" % i, [128, D], F32) for i in range(2)]
    tmp = T("wtmp", [128, D], F32)
    hb = [T("whb%d" % i, [128, D], BF16) for i in range(2)]
    junk = T("wjunk", [128, D], BF16)
    ss = T("wss", [128, 32], F32)
    hT = [T("whT%d" % i, [128, KC, 128], BF16) for i in range(2)]
    lg = T("lg", [128, NE], F32); mx = T("mx", [128, 1], F32); sm = T("sm", [128, 1], F32)
    aff = T("aff", [128, NE], F32)
    affT = T("affT", [NE, NX], F32)
    mv = G.mixT_d.rearrange("(k p) t -> p k t", p=128)
    ident = G.ident
    lgs = [lg, T("lg1", [128, NE], F32)]
    mxs = [mx, T("mx1", [128, 1], F32)]
    sms = [sm, T("sm1", [128, 1], F32)]
    affs = [aff, T("aff1", [128, NE], F32)]

    def s_load(t):
        s = t % 2; r0 = t * 128
        S.dma("sp", lambda e: e.dma_start(out=mT[s][:], in_=mv[:, :, r0:r0 + 128]),
              reads=[("mixT_d", x_) for x_ in range(8)] + [("mixT_d", 8 + t)], writes=[("mT", s)])
        S.dma("sp", lambda e: e.dma_start(out=xt[s][:], in_=G.x[r0:r0 + 128, :]), writes=[("wxt", s)])

    def s_main(t):
        s = t % 2; r0 = t * 128
        for cb in range(4):
            for k in range(KC):
                S.op("pe", lambda e, k=k, cb=cb: e.matmul(G.ps[cb][:, :], lhsT=mT[s][:, k, :], rhs=wo[:, k, cb * 512:(cb + 1) * 512],
                                                       start=(k == 0), stop=(k == KC - 1)),
                     reads=[("mT", s), ("wo", cb)], writes=[("ps", cb)])
            cs = slice(cb * 512, (cb + 1) * 512)
            S.op("dve", lambda e, cb=cb, cs=cs: e.tensor_tensor(out=x1[s][:, cs], in0=G.ps[cb][:, :], in1=gt1[:, cs], op=ALU.mult),
                 reads=[("ps", cb), "gt1"], writes=[("x1", s, cb)])
            S.op("pool", lambda e, cs=cs: e.tensor_tensor(out=x1[s][:, cs], in0=x1[s][:, cs], in1=xt[s][:, cs], op=ALU.add),
                 reads=[("x1", s, cb), ("wxt", s)], writes=[("x1", s, cb)])
        x1k = [("x1", s, cb) for cb in range(4)]
        S.dma("pool", lambda e: e.dma_start(out=G.x1_d[r0:r0 + 128, :], in_=x1[s][:]), reads=x1k, writes=["x1_d"])

    def s_norm(t):
        s = t % 2; r0 = t * 128
        x1k = [("x1", s, cb) for cb in range(4)]
        S.op("act", lambda e: e.activation(out=junk[:], in_=x1[s][:], func=AF.Square, scale=D ** -0.5, accum_out=ss[:, t:t + 1]),
             reads=x1k, writes=["wjunk", ("wss", t)])
        S.op("act", lambda e: e.activation(out=ss[:, t:t + 1], in_=ss[:, t:t + 1], func=AF.Sqrt, bias=EPS, scale=1.0), reads=[("wss", t)], writes=[("wss", t)])
        S.op("dve", lambda e: e.reciprocal(out=ss[:, t:t + 1], in_=ss[:, t:t + 1]), reads=[("wss", t)], writes=[("wss", t)])
        S.op("dve", lambda e: e.scalar_tensor_tensor(out=tmp[:], in0=x1[s][:], scalar=ss[:, t:t + 1], in1=gm2[:], op0=ALU.mult, op1=ALU.mult),
             reads=x1k + [("wss", t), "gm2"], writes=["wtmp"])
        S.op("pool", lambda e: e.tensor_tensor(out=hb[s][:], in0=tmp[:], in1=sh2[:], op=ALU.add), reads=["wtmp", "sh2"], writes=[("whb", s)])
        S.dma("pool", lambda e: e.dma_start(out=G.hx2_d[r0:r0 + 128, :], in_=hb[s][:]), reads=[("whb", s)], writes=["hx2_d"])

    def s_tr(t):
        s = t % 2
        for half in range(2):
            bk = 6 + half
            for j in range(8):
                k = half * 8 + j
                S.op("pe", lambda e, k=k, j=j, half=half: e.transpose(out=G.pst[half][:, j * 128:(j + 1) * 128], in_=hb[s][:, k * 128:(k + 1) * 128], identity=ident[:]),
                     reads=[("whb", s), "ident"], writes=[("ps", bk)])
            if half == 0:
                S.op("act", lambda e: e.activation(out=hT[s][:, 0:8, :], in_=G.pst[0][:, :].rearrange("p (k t) -> p k t", k=8), func=AF.Copy),
                     reads=[("ps", bk)], writes=[("whT", s, 0)])
            else:
                S.op("dve", lambda e: e.tensor_copy(out=hT[s][:, 8:16, :], in_=G.pst[1][:, :].rearrange("p (k t) -> p k t", k=8)),
                     reads=[("ps", bk)], writes=[("whT", s, 1)])

    def s_route(t):
        s = t % 2
        lg_, mx_, sm_, aff_ = lgs[s], mxs[s], sms[s], affs[s]
        for k in range(KC):
            S.op("pe", lambda e, k=k: e.matmul(G.ps[4][:, 0:NE], lhsT=hT[s][:, k, :], rhs=wr[:, k, :], start=(k == 0), stop=(k == KC - 1)),
                 reads=[("whT", s, 0), ("whT", s, 1), "wr"], writes=[("ps", 4)])
        S.op("act", lambda e: e.activation(out=lg_[:], in_=G.ps[4][:, 0:NE], func=AF.Copy), reads=[("ps", 4)], writes=[("lg", s)])
        S.op("dve", lambda e: e.tensor_reduce(out=mx_[:], in_=lg_[:], axis=AX.X, op=ALU.max), reads=[("lg", s)], writes=[("mx", s)])
        S.op("dve", lambda e: e.tensor_scalar(out=mx_[:], in0=mx_[:], scalar1=-1.0, scalar2=None, op0=ALU.mult), reads=[("mx", s)], writes=[("mx", s)])
        S.op("act", lambda e: e.activation(out=aff_[:], in_=lg_[:], func=AF.Exp, bias=mx_[:, 0:1], scale=1.0, accum_out=sm_[:, 0:1]), reads=[("lg", s), ("mx", s)], writes=[("aff", s), ("sm", s)])
        S.op("dve", lambda e: e.reciprocal(out=sm_[:], in_=sm_[:]), reads=[("sm", s)], writes=[("sm", s)])
        S.op("dve", lambda e: e.tensor_scalar(out=aff_[:], in0=aff_[:], scalar1=sm_[:, 0:1], scalar2=None, op0=ALU.mult), reads=[("aff", s), ("sm", s)], writes=[("aff", s)])

    def s_afft(t):
        s = t % 2; r0 = t * 128
        aff_ = affs[s]
        S.op("pe", lambda e: e.transpose(out=G.ps[5][0:NE, 0:128], in_=aff_[:], identity=identf[:]), reads=[("aff", s), "identf"], writes=[("ps", 5)])
        S.op("act", lambda e: e.activation(out=affT[:, r0:r0 + 128], in_=G.ps[5][0:NE, 0:128], func=AF.Copy), reads=[("ps", 5)], writes=[("affT", t)])

    stages = [s_load, s_main, s_norm, s_tr, s_route, s_afft]
    for i in range(32 + len(stages) - 1):
        for k_, st in enumerate(stages):
            j = i - k_
            if 0 <= j < 32:
                st(j)
    S.dma("sp", lambda e: e.dma_start(out=G.affT_d, in_=affT[:]), reads=[("affT", t) for t in range(32)], writes=["affT_d"])


def phaseK_topk(G, S, es, keep):
    nc = G.nc
    def T(name, shape, dt, st=es):
        return st.enter_context(nc.sbuf_tensor(name, shape, dt))
    G.idxT = T("idxT", [128, 4, NE], I32, keep)
    G.valsT = T("valsT", [128, 4, NE], F32, keep)
    NSEG = 8
    NCAND = 104
    orig = T("orig", [NE, NX], F32)
    w128 = T("w128", [128, NX // NSEG], F32)
    cv = T("cv", [128, NCAND], F32)
    c16 = T("c16", [NE, NSEG * NCAND], F32)
    vals = T("vals", [NE, CAP], F32)
    idxs = T("idxs", [NE, CAP], U32)
    idxf = T("idxf", [NE, CAP], F32)
    identf = T("identf2", [128, 128], F32)
    S.dma("sp", lambda e: e.dma_start(out=identf[:], in_=G.ident_f), writes=["identf2"])
    S.dma("sp", lambda e: e.dma_start(out=orig[:], in_=G.affT_d), reads=["affT_d"], writes=["orig"])
    S.dma("sp", lambda e: e.dma_start(out=w128[:], in_=G.affT_d.rearrange("e (s t) -> (e s) t", s=NSEG)), reads=["affT_d"], writes=["w128"])
    for r in range(NCAND // 8):
        rs = slice(r * 8, (r + 1) * 8)
        S.op("dve", lambda e, rs=rs: e.max(out=cv[:, rs], in_=w128[:]), reads=["w128"], writes=[("cv", r)])
        S.op("dve", lambda e, rs=rs: e.match_replace(out=w128[:], in_to_replace=cv[:, rs], in_values=w128[:], imm_value=-1.0),
             reads=["w128", ("cv", r)], writes=["w128"])
    S.dma("sp", lambda e: e.dma_start(out=G.cand_d, in_=cv[:]), reads=[("cv", r) for r in range(NCAND // 8)], writes=["cand_d"])
    S.dma("sp", lambda e: e.dma_start(out=c16[:], in_=G.cand_d.rearrange("(e s) j -> e (s j)", s=NSEG)), reads=["cand_d"], writes=["c16"])
    for r in range(CAP // 8):
        rs = slice(r * 8, (r + 1) * 8)
        S.op("dve", lambda e, rs=rs: e.max(out=vals[:, rs], in_=c16[:]), reads=["c16"], writes=[("vals", r)])
        S.op("dve", lambda e, rs=rs: e.match_replace(out=c16[:], in_to_replace=vals[:, rs], in_values=c16[:], imm_value=-1.0),
             reads=["c16", ("vals", r)], writes=["c16"])
    for r in range(CAP // 8):
        rs = slice(r * 8, (r + 1) * 8)
        S.op("dve", lambda e, rs=rs: e.max_index(out=idxs[:, rs], in_max=vals[:, rs], in_values=orig[:]), reads=["orig", ("vals", r)], writes=[("idxs", r)])
    allv = [("vals", r) for r in range(CAP // 8)]; alli = [("idxs", r) for r in range(CAP // 8)]
    S.op("dve", lambda e: e.tensor_copy(out=idxf[:], in_=idxs[:]), reads=alli, writes=["idxf"])
    for t in range(4):
        S.op("pe", lambda e, t=t: e.transpose(out=G.ps[0][:, t * NE:(t + 1) * NE], in_=idxf[:, t * 128:(t + 1) * 128], identity=identf[0:NE, 0:NE]),
             reads=["idxf", "identf2"], writes=[("ps", 0)])
        S.op("pe", lambda e, t=t: e.transpose(out=G.ps[1][:, t * NE:(t + 1) * NE], in_=vals[:, t * 128:(t + 1) * 128], identity=identf[0:NE, 0:NE]),
             reads=allv + ["identf2"], writes=[("ps", 1)])
    S.op("dve", lambda e: e.tensor_copy(out=G.idxT[:], in_=G.ps[0][:, 0:4 * NE].rearrange("p (t e) -> p t e", e=NE)), reads=[("ps", 0)], writes=["idxT"])
    S.op("act", lambda e: e.activation(out=G.valsT[:], in_=G.ps[1][:, 0:4 * NE].rearrange("p (t e) -> p t e", e=NE), func=AF.Copy), reads=[("ps", 1)], writes=["valsT"])


def phaseM_moe(G, S, es):
    nc = G.nc
    def T(name, shape, dt):
        return es.enter_context(nc.sbuf_tensor(name, shape, dt))
    gt2 = T("gt2", [128, D], F32)
    S.dma("sp", lambda e: e.dma_start(out=gt2[:], in_=bc(G.mod_d[0:1, 5 * D:6 * D], D)), reads=["mod_d"], writes=["gt2"])
    xg = [T("xg%d" % i, [128, D], BF16) for i in range(2)]
    xgT = [T("xgT%d" % i, [128, KC, CAP], BF16) for i in range(2)]
    wg = [T("wg%d" % i, [128, KC, 512], BF16) for i in range(2)]
    wu = [T("wu%d" % i, [128, KC, 512], BF16) for i in range(2)]
    wd = [T("wd%d" % i, [128, 12, 512], BF16) for i in range(2)]
    hid = T("hid", [128, 12, CAP], BF16)
    sg = [T("sg%d" % i, [128, CAP], F32) for i in range(2)]
    yt = [T("yt%d" % i, [128, D], F32) for i in range(4)]
    ident = G.ident
    cnt = {"nw": 0, "nd": 0, "ne": 0}

    def gather_tr(ex):
        xs = ex % 2
        for t in range(4):
            gs = (ex * 4 + t) % 2
            S.dma("pool", lambda e, gs=gs, t=t, ex=ex: e.indirect_dma_start(
                out=xg[gs][:], out_offset=None, in_=G.hx2_d[:, :],
                in_offset=bass.IndirectOffsetOnAxis(ap=G.idxT[:, t, ex:ex + 1], axis=0)),
                reads=["hx2_d", "idxT"], writes=[("xg", gs)])
            for half in range(2):
                bk = 6 + half
                for j in range(8):
                    k = half * 8 + j
                    S.op("pe", lambda e, gs=gs, k=k, j=j, half=half: e.transpose(out=G.pst[half][:, j * 128:(j + 1) * 128], in_=xg[gs][:, k * 128:(k + 1) * 128], identity=ident[:]),
                         reads=[("xg", gs), "ident"], writes=[("ps", bk)])
                if half == 0:
                    S.op("act", lambda e, xs=xs, t=t: e.activation(out=xgT[xs][:, 0:8, t * 128:(t + 1) * 128], in_=G.pst[0][:, :].rearrange("p (k t) -> p k t", k=8), func=AF.Copy),
                         reads=[("ps", bk)], writes=[("xgT", xs, t, 0)])
                else:
                    S.op("dve", lambda e, xs=xs, t=t: e.tensor_copy(out=xgT[xs][:, 8:16, t * 128:(t + 1) * 128], in_=G.pst[1][:, :].rearrange("p (k t) -> p k t", k=8)),
                         reads=[("ps", bk)], writes=[("xgT", xs, t, 1)])

    def gateup(ex):
        xs = ex % 2
        xk = [("xgT", xs, t, hh) for t in range(4) for hh in range(2)]
        gv = G.w_gate[ex].rearrange("(k p) f -> p k f", p=128)
        uv = G.w_up[ex].rearrange("(k p) f -> p k f", p=128)
        for fb in range(3):
            ws = cnt["nw"] % 2
            cnt["nw"] += 1
            S.dma("pool", lambda e, ws=ws, fb=fb, gv=gv: e.dma_start(out=wg[ws][:], in_=gv[:, :, fb * 512:(fb + 1) * 512]), writes=[("wg", ws)])
            S.dma("pool", lambda e, ws=ws, fb=fb, uv=uv: e.dma_start(out=wu[ws][:], in_=uv[:, :, fb * 512:(fb + 1) * 512]), writes=[("wu", ws)])
            for c4 in range(4):
                fc = fb * 4 + c4
                es_ = cnt["ne"] % 2
                cnt["ne"] += 1
                pg = G.ps[es_ * 2]; pu = G.ps[es_ * 2 + 1]
                for k in range(KC):
                    S.op("pe", lambda e, ws=ws, k=k, c4=c4, xs=xs, pg=pg: e.matmul(pg[:, :], lhsT=wg[ws][:, k, c4 * 128:(c4 + 1) * 128], rhs=xgT[xs][:, k, :],
                                                                               start=(k == 0), stop=(k == KC - 1)),
                         reads=[("wg", ws)] + xk, writes=[("ps", es_ * 2)])
                for k in range(KC):
                    S.op("pe", lambda e, ws=ws, k=k, c4=c4, xs=xs, pu=pu: e.matmul(pu[:, :], lhsT=wu[ws][:, k, c4 * 128:(c4 + 1) * 128], rhs=xgT[xs][:, k, :],
                                                                               start=(k == 0), stop=(k == KC - 1)),
                         reads=[("wu", ws)] + xk, writes=[("ps", es_ * 2 + 1)])
                S.op("act", lambda e, es_=es_, pg=pg: e.activation(out=sg[es_][:], in_=pg[:, :], func=AF.Silu), reads=[("ps", es_ * 2)], writes=[("sg", es_)])
                S.op("dve", lambda e, es_=es_, pu=pu, fc=fc: e.tensor_tensor(out=hid[:, fc, :], in0=sg[es_][:], in1=pu[:, :], op=ALU.mult),
                     reads=[("ps", es_ * 2 + 1), ("sg", es_)], writes=[("hid", fc)])

    def down(ex):
        dv = G.w_down[ex].rearrange("(k p) n -> p k n", p=128)
        hk = [("hid", fc) for fc in range(12)]
        for cb in range(4):
            ds = cnt["nd"] % 2
            cnt["nd"] += 1
            S.dma("pool", lambda e, ds=ds, cb=cb, dv=dv: e.dma_start(out=wd[ds][:], in_=dv[:, :, cb * 512:(cb + 1) * 512]), writes=[("wd", ds)])
            for t in range(4):
                pb = 4 + (t % 2)
                for fc in range(12):
                    S.op("pe", lambda e, ds=ds, fc=fc, t=t, pb=pb: e.matmul(G.ps[pb][:, :], lhsT=hid[:, fc, t * 128:(t + 1) * 128], rhs=wd[ds][:, fc, :],
                                                                        start=(fc == 0), stop=(fc == 11)),
                         reads=[("wd", ds)] + hk, writes=[("ps", pb)])
                cs = slice(cb * 512, (cb + 1) * 512)
                S.op("dve", lambda e, t=t, pb=pb, cs=cs, ex=ex: e.scalar_tensor_tensor(out=yt[t][:, cs], in0=G.ps[pb][:, :], scalar=G.valsT[:, t, ex:ex + 1], in1=gt2[:, cs],
                                                                               op0=ALU.mult, op1=ALU.mult),
                     reads=[("ps", pb), "valsT", "gt2"], writes=[("yt", t, cb)])

    def scatter(ex):
        for t in range(4):
            S.dma("pool", lambda e, t=t, ex=ex: e.indirect_dma_start(
                out=G.x1_d[:, :], out_offset=bass.IndirectOffsetOnAxis(ap=G.idxT[:, t, ex:ex + 1], axis=0),
                in_=yt[t][:], in_offset=None, compute_op=ALU.add),
                reads=[("yt", t, cb) for cb in range(4)] + ["idxT", "x1_d"], writes=["x1_d"])

    gather_tr(0)
    gateup(0)
    for ex in range(NE):
        if ex + 1 < NE:
            gather_tr(ex + 1)
        down(ex)
        if ex + 1 < NE:
            gateup(ex + 1)
        scatter(ex)


def phaseZ_final(G, S, es):
    nc = G.nc
    def T(name, shape, dt):
        return es.enter_context(nc.sbuf_tensor(name, shape, dt))
    nf = T("nf", [128, D], F32)
    S.dma("sp", lambda e: e.dma_start(out=nf[:], in_=bc(G.norm_f[0:1, :], D)), writes=["nf"])
    xt = [T("zx%d" % i, [128, D], F32) for i in range(2)]
    junk = T("zjunk", [128, D], BF16)
    ss = T("zss", [128, 32], F32)
    for t in range(32):
        s = t % 2
        r0 = t * 128
        S.dma("sp", lambda e, s=s, r0=r0: e.dma_start(out=xt[s][:], in_=G.x1_d[r0:r0 + 128, :]), reads=["x1_d"], writes=[("zx", s)])
        S.op("act", lambda e, s=s, t=t: e.activation(out=junk[:], in_=xt[s][:], func=AF.Square, scale=D ** -0.5, accum_out=ss[:, t:t + 1]),
             reads=[("zx", s)], writes=["zjunk", ("zss", t)])
        S.op("act", lambda e, t=t: e.activation(out=ss[:, t:t + 1], in_=ss[:, t:t + 1], func=AF.Sqrt, bias=EPS, scale=1.0), reads=[("zss", t)], writes=[("zss", t)])
        S.op("dve", lambda e, t=t: e.reciprocal(out=ss[:, t:t + 1], in_=ss[:, t:t + 1]), reads=[("zss", t)], writes=[("zss", t)])
        S.op("dve", lambda e, s=s, t=t: e.scalar_tensor_tensor(out=xt[s][:], in0=xt[s][:], scalar=ss[:, t:t + 1], in1=nf[:], op0=ALU.mult, op1=ALU.mult),
             reads=[("zx", s), ("zss", t), "nf"], writes=[("zx", s)])
        S.dma("pool", lambda e, s=s, r0=r0: e.dma_start(out=G.out[r0:r0 + 128, :], in_=xt[s][:]), reads=[("zx", s)], writes=[("out", t)])


from concourse.bass_utils import run_bass_kernel_spmd
import ml_dtypes

_DBG = ()
N_CORES = 8
_PHASES = None


def build_program(dbg=()):
    nc = bass.Bass("TRN2", target_bir_lowering=False)
    G = declare(nc, dbg)
    declareB(G, nc, dbg); declareG(G, nc, dbg); declareF(G, nc, dbg); declareW(G, nc, dbg)
    with ExitStack() as es:
        S = Sched(nc, es)
        G.ps = [es.enter_context(nc.psum_tensor("ps%d" % i, [128, 512], F32)) for i in range(8)]
        G.pst = [G.ps[6 + i][:].bitcast(BF16) for i in range(2)]
        G.psh = [G.ps[4 + i][:].bitcast(F16) for i in range(2)]
        G.ident = es.enter_context(nc.sbuf_tensor("ident", [128, 128], BF16))
        S.dma("sp", lambda e: e.dma_start(out=G.ident[:], in_=G.ident_bf), writes=["ident"])
        keep = es
        phases = [("0", lambda G, S, es: [None for _ in phase0_mod(G, S, es)]), ("A", lambda G, S, es: [None for _ in phaseA_modulate(G, S, es)]), ("B1", phaseB1_qkv), ("B2", phaseB2_fz), ("G", phaseG_gdn),
                  ("F", phaseF_fourier), ("O", phaseO_gdnout), ("W", phaseW_out), ("K", None), ("M", phaseM_moe), ("Z", phaseZ_final)]
        for name, ph in phases:
            if _PHASES is not None and name not in _PHASES:
                continue
            with ExitStack() as es2:
                if name == "K":
                    phaseK_topk(G, S, es2, keep)
                else:
                    ph(G, S, es2)
            S.barrier()
        S.final_wait("sp", list(S.res.keys()))
        S.emit()
    return nc


def make_in_maps(inputs, n_cores=8):
    f = lambda a: np.ascontiguousarray(np.asarray(a, dtype=np.float32))
    W1, W2, W3 = fourier_tables()
    conv_w = f(inputs["conv_w"])[0]
    shared = {
        "w_mod": f(inputs["w_mod"])[0], "b_mod": f(inputs["b_mod"]), "norm1_g": f(inputs["norm1_g"]), "norm2_g": f(inputs["norm2_g"]),
        "w_in": f(inputs["w_in"])[0],
        "ident_bf": np.eye(128).astype(ml_dtypes.bfloat16), "ident_h": np.eye(128).astype(np.float16),
        "ident_f": np.eye(128, dtype=np.float32), "ones_f": np.ones((128, 128), np.float32),
        "conv_wT": np.ascontiguousarray(conv_w.reshape(5, 24, 128).transpose(2, 1, 0)),
        "ab_par": np.concatenate([f(inputs["a_log"])[0].reshape(-1), f(inputs["dt_bias"])[0].reshape(-1)])[None, :].astype(np.float32),
        "cmask": gdn_masks(), "W1": W1, "W2": W2, "W3": W3, "gnw": f(inputs["gdn_norm_w"]),
        "w_out": f(inputs["w_out"])[0], "w_router": f(inputs["w_router"])[0],
        "w_gate": f(inputs["w_gate"])[0], "w_up": f(inputs["w_up"])[0], "w_down": f(inputs["w_down"])[0],
        "norm_f": f(inputs["norm_f"])[None, :],
    }
    x = f(inputs["x"]); c = f(inputs["c"]); ctx = f(inputs["ctx"]); c_ctx = f(inputs["c_ctx"])
    maps = []
    for i in range(n_cores):
        b = i % 4
        cc = np.stack([c[b], c_ctx])
        m = dict(shared)
        m["x"] = x[b]; m["ctx"] = ctx[b]
        m["ccT"] = np.ascontiguousarray(cc.reshape(2, 16, 128).transpose(2, 1, 0))
        maps.append(m)
    return maps


def kernel(**inputs):
    nc = build_program(_DBG)
    maps = make_in_maps(inputs, N_CORES)
    res = run_bass_kernel_spmd(nc, maps, core_ids=list(range(N_CORES)))
    out = np.stack([np.asarray(res.results[b]["out"], dtype=np.float32) for b in range(4)], 0)
    return out
```

```python
import numpy as np
from contextlib import ExitStack
import concourse.bass as bass
import concourse.mybir as mybir

F32 = mybir.dt.float32
BF16 = mybir.dt.bfloat16
F16 = mybir.dt.float16
I32 = mybir.dt.int32
U32 = mybir.dt.uint32
AF = mybir.ActivationFunctionType
ALU = mybir.AluOpType
AX = mybir.AxisListType

ENGS = ("pe", "act", "dve", "pool", "sp")
N_DMA_SEMS = 48


class Sched:
    def __init__(self, nc, es):
        self.nc = nc
        self.streams = {e: [] for e in ENGS}
        self.esem = {e: es.enter_context(nc.semaphore("s_" + e)) for e in ENGS if e != "sp"}
        self.dsem = [es.enter_context(nc.semaphore("d%d" % i)) for i in range(N_DMA_SEMS)]
        self.count = {e: 0 for e in ENGS}
        self.dcount = [0] * N_DMA_SEMS
        self.dnext = 0
        self.waited = {e: {} for e in ENGS}
        self.res = {}
        self.same_engine_waits = True

    def _sem(self, key):
        return self.esem[key[1]] if key[0] == "e" else self.dsem[key[1]]

    def _deps(self, reads, writes):
        deps = {}
        def add(tok):
            if tok is None:
                return
            k, v = tok
            if deps.get(k, 0) < v:
                deps[k] = v
        for r in reads:
            st = self.res.get(r)
            if st:
                add(st["w"])
        for w in writes:
            st = self.res.get(w)
            if st:
                add(st["w"])
                for k, v in st["r"].items():
                    add((k, v))
        return deps

    def _update(self, reads, writes, tok):
        for r in reads:
            st = self.res.setdefault(r, {"w": None, "r": {}})
            k, v = tok
            if st["r"].get(k, 0) < v:
                st["r"][k] = v
        for w in writes:
            self.res[w] = {"w": tok, "r": {}}

    def _waits(self, eng, deps):
        ws = []
        for k, v in deps.items():
            if k == ("e", eng) and (eng == "pe" or not self.same_engine_waits):
                continue
            if self.waited[eng].get(k, 0) < v:
                self.waited[eng][k] = v
                ws.append((k, v))
        return ws

    def op(self, eng, fn, reads=(), writes=()):
        deps = self._deps(reads, writes)
        ws = self._waits(eng, deps)
        self.count[eng] += 1
        tok = (("e", eng), self.count[eng])
        self.streams[eng].append((ws, fn, tok[0], 1))
        self._update(reads, writes, tok)

    def dma(self, eng, fn, reads=(), writes=()):
        deps = self._deps(reads, writes)
        i = self.dnext
        self.dnext = (self.dnext + 1) % N_DMA_SEMS
        if self.dcount[i] > 0:
            k = ("d", i)
            if deps.get(k, 0) < self.dcount[i]:
                deps[k] = self.dcount[i]
        ws = self._waits(eng, deps)
        self.dcount[i] += 16
        tok = (("d", i), self.dcount[i])
        self.streams[eng].append((ws, fn, tok[0], 16))
        self._update(reads, writes, tok)
        return tok

    def barrier(self):
        deps = {}
        for e in ENGS:
            if e != "sp" and self.count[e] > 0:
                deps[("e", e)] = self.count[e]
        for i in range(N_DMA_SEMS):
            if self.dcount[i] > 0:
                deps[("d", i)] = self.dcount[i]
        for e in ENGS:
            ws = []
            for k, v in deps.items():
                if k == ("e", e):
                    continue
                if self.waited[e].get(k, 0) < v:
                    self.waited[e][k] = v
                    ws.append((k, v))
            self.streams[e].append((ws, None, None, 0))

    def final_wait(self, eng, resources):
        deps = self._deps(resources, resources)
        ws = self._waits(eng, deps)
        self.streams[eng].append((ws, None, None, 0))

    def emit(self):
        nc = self.nc
        with nc.Block() as block:
            def mk(name):
                def body(e):
                    for ws, fn, key, inc in self.streams[name]:
                        for k, v in ws:
                            e.wait_ge(self._sem(k), v)
                        if fn is not None:
                            ins = fn(e)
                            ins.then_inc(self._sem(key), inc)
                return body
            block.tensor(mk("pe"))
            block.scalar(mk("act"))
            block.vector(mk("dve"))
            block.gpsimd(mk("pool"))
            block.sync(mk("sp"))


D = 2048
KC = 16
NX = 4096
NCTX = 256
NT = NX + NCTX
IN_COLS = 5152
EPS = 1e-6


def bc(ap_row, n, parts=128):
    return bass.AP(ap_row.tensor, ap_row.offset, [[0, parts], [1, n]])


class Ctx:
    pass


def declare(nc, dbg=()):
    G = Ctx()
    G.nc = nc
    def inp(name, shape, dt=F32):
        return nc.dram_tensor(name, list(shape), dt, kind="ExternalInput").ap()
    def scr(name, shape, dt):
        kind = "ExternalOutput" if name in dbg else "Internal"
        return nc.dram_tensor(name, list(shape), dt, kind=kind).ap()
    G.x = inp("x", [NX, D])
    G.ctx = inp("ctx", [NCTX, D])
    G.ccT = inp("ccT", [128, KC, 2])
    G.w_mod = inp("w_mod", [D, 6 * D])
    G.b_mod = inp("b_mod", [1, 6 * D])
    G.norm1_g = inp("norm1_g", [1, D])
    G.norm2_g = inp("norm2_g", [1, D])
    G.w_in = inp("w_in", [D, IN_COLS])
    G.ident_bf = inp("ident_bf", [128, 128], BF16)
    G.out = nc.dram_tensor("out", [NX, D], F32, kind="ExternalOutput").ap()
    G.mod_d = scr("mod_d", [2, 6 * D], F32)
    G.hT_d = scr("hT_d", [D, NT], BF16)
    return G


def phase0_mod(G, S, es):
    nc = G.nc
    cT = es.enter_context(nc.sbuf_tensor("cT", [128, KC, 2], F32))
    sT = es.enter_context(nc.sbuf_tensor("sT", [128, KC, 2], BF16))
    wm = [es.enter_context(nc.sbuf_tensor("wm%d" % i, [128, KC, 512], BF16)) for i in range(2)]
    bm = [es.enter_context(nc.sbuf_tensor("bm%d" % i, [2, 512], F32)) for i in range(2)]
    gg = [es.enter_context(nc.sbuf_tensor("gg%d" % i, [2, 512], F32)) for i in range(2)]
    row = [es.enter_context(nc.sbuf_tensor("row%d" % i, [2, 512], F32)) for i in range(2)]
    S.dma("sp", lambda e: e.dma_start(out=cT[:], in_=G.ccT), writes=["cT"])
    S.op("act", lambda e: e.activation(out=sT[:], in_=cT[:], func=AF.Silu), reads=["cT"], writes=["sT"])
    wv = G.w_mod.rearrange("(k p) n -> p k n", p=128)
    for j in range(24):
        s = j % 2
        sec = j // 4
        c0 = j * 512
        S.dma("pool", lambda e, s=s, c0=c0: e.dma_start(out=wm[s][:], in_=wv[:, :, c0:c0 + 512]),
              writes=[("wm", s)])
        S.dma("sp", lambda e, s=s, c0=c0: e.dma_start(out=bm[s][:], in_=bc(G.b_mod[0:1, c0:c0 + 512], 512, 2)),
              writes=[("bm", s)])
        if sec in (1, 4):
            gsrc = G.norm1_g if sec == 1 else G.norm2_g
            g0 = c0 - sec * D
            S.dma("sp", lambda e, s=s, g0=g0, gsrc=gsrc: e.dma_start(out=gg[s][:], in_=bc(gsrc[0:1, g0:g0 + 512], 512, 2)),
                  writes=[("gg", s)])
        pb = G.ps[j % 2]
        for k in range(KC):
            S.op("pe", lambda e, s=s, k=k, pb=pb: e.matmul(pb[0:2, :], lhsT=sT[:, k, :], rhs=wm[s][:, k, :],
                                                       start=(k == 0), stop=(k == KC - 1)),
                 reads=["sT", ("wm", s)], writes=[("ps", j % 2)])
        S.op("dve", lambda e, s=s, pb=pb: e.tensor_tensor(out=row[s][:], in0=pb[0:2, :], in1=bm[s][:], op=ALU.add),
             reads=[("ps", j % 2), ("bm", s)], writes=[("row", s)])
        if sec in (1, 4):
            S.op("dve", lambda e, s=s: e.scalar_tensor_tensor(out=row[s][:], in0=row[s][:], scalar=1.0, in1=gg[s][:],
                                                              op0=ALU.add, op1=ALU.mult),
                 reads=[("row", s), ("gg", s)], writes=[("row", s)])
        S.dma("act", lambda e, s=s, c0=c0: e.dma_start(out=G.mod_d[:, c0:c0 + 512], in_=row[s][:]),
              reads=[("row", s)], writes=["mod_d"])
        yield


def phaseA_modulate(G, S, es):
    nc = G.nc
    gm = {}
    for nm in ("gmx", "shx", "gmc", "shc"):
        gm[nm] = es.enter_context(nc.sbuf_tensor(nm, [128, D], F32))
    S.dma("sp", lambda e: e.dma_start(out=gm["shx"][:], in_=bc(G.mod_d[0:1, 0:D], D)), reads=["mod_d"], writes=["shx"])
    S.dma("sp", lambda e: e.dma_start(out=gm["gmx"][:], in_=bc(G.mod_d[0:1, D:2 * D], D)), reads=["mod_d"], writes=["gmx"])
    S.dma("sp", lambda e: e.dma_start(out=gm["shc"][:], in_=bc(G.mod_d[1:2, 0:D], D)), reads=["mod_d"], writes=["shc"])
    S.dma("sp", lambda e: e.dma_start(out=gm["gmc"][:], in_=bc(G.mod_d[1:2, D:2 * D], D)), reads=["mod_d"], writes=["gmc"])
    xt = [es.enter_context(nc.sbuf_tensor("xt%d" % i, [128, D], F32)) for i in range(4)]
    h1 = [es.enter_context(nc.sbuf_tensor("h1_%d" % i, [128, D], F32)) for i in range(2)]
    hb = [es.enter_context(nc.sbuf_tensor("hb%d" % i, [128, D], BF16)) for i in range(2)]
    junk = es.enter_context(nc.sbuf_tensor("junkA", [128, D], BF16))
    ss = es.enter_context(nc.sbuf_tensor("ssA", [128, 64], F32))
    hTb = [es.enter_context(nc.sbuf_tensor("hTb%d" % i, [128, KC, 512], BF16)) for i in range(2)]
    ident = G.ident
    hv = G.hT_d.rearrange("(k p) t -> p k t", p=128)
    ntiles = NT // 128

    def info(t):
        isctx = t < 2
        if isctx:
            return isctx, 0, t, 256
        return isctx, 1 + (t - 2) // 4, (t - 2) % 4, 512

    def a_load(t):
        s3 = t % 4
        src = G.ctx[t * 128:(t + 1) * 128, :] if t < 2 else G.x[(t - 2) * 128:(t - 1) * 128, :]
        S.dma("sp", lambda e: e.dma_start(out=xt[s3][:], in_=src), writes=[("xt", s3)])

    def a_stat(t):
        s3 = t % 4
        S.op("act", lambda e: e.activation(out=junk[:], in_=xt[s3][:], func=AF.Square, scale=D ** -0.5, accum_out=ss[:, t:t + 1]),
             reads=[("xt", s3)], writes=["junkA", ("ssA", t)])
        S.op("act", lambda e: e.activation(out=ss[:, t:t + 1], in_=ss[:, t:t + 1], func=AF.Sqrt, bias=EPS, scale=1.0), reads=[("ssA", t)], writes=[("ssA", t)])
        S.op("dve", lambda e: e.reciprocal(out=ss[:, t:t + 1], in_=ss[:, t:t + 1]), reads=[("ssA", t)], writes=[("ssA", t)])

    def a_mod(t):
        s3 = t % 4; s = t % 2
        isctx = t < 2
        g_, s_ = ("gmc", "shc") if isctx else ("gmx", "shx")
        S.op("dve", lambda e: e.scalar_tensor_tensor(out=h1[s][:], in0=xt[s3][:], scalar=ss[:, t:t + 1], in1=gm[g_][:], op0=ALU.mult, op1=ALU.mult),
             reads=[("xt", s3), ("ssA", t), g_], writes=[("h1", s)])
        S.op("pool", lambda e: e.tensor_tensor(out=hb[s][:], in0=h1[s][:], in1=gm[s_][:], op=ALU.add), reads=[("h1", s), s_], writes=[("hb", s)])

    def a_tr(t):
        s = t % 2
        isctx, bi, tt, bw = info(t)
        bs = bi % 2
        for half in range(2):
            pt = G.pst[half]
            for j in range(8):
                k = half * 8 + j
                S.op("pe", lambda e, k=k, j=j, pt=pt: e.transpose(out=pt[:, j * 128:(j + 1) * 128], in_=hb[s][:, k * 128:(k + 1) * 128], identity=ident[:]),
                     reads=[("hb", s), "ident"], writes=[("ps", 6 + half)])
            o = hTb[bs][:, half * 8:(half + 1) * 8, tt * 128:(tt + 1) * 128]
            i_ = pt[:, :].rearrange("p (k t) -> p k t", k=8)
            if half == 0:
                S.op("act", lambda e, o=o, i_=i_: e.activation(out=o, in_=i_, func=AF.Copy), reads=[("ps", 6)], writes=[("hTb", bs, tt, 0)])
            else:
                S.op("dve", lambda e, o=o, i_=i_: e.tensor_copy(out=o, in_=i_), reads=[("ps", 7)], writes=[("hTb", bs, tt, 1)])
        last = (isctx and tt == 1) or ((not isctx) and tt == 3)
        if last:
            t0 = 0 if isctx else NCTX + (bi - 1) * 512
            S.dma("sp", lambda e: e.dma_start(out=hv[:, :, t0:t0 + bw], in_=hTb[bs][:, :, 0:bw]),
                  reads=[("hTb", bs, a_, b_) for a_ in range(4) for b_ in range(2)], writes=[("hT_d", bi)])

    stages = [(0, a_load), (2, a_stat), (3, a_mod), (4, a_tr)]
    for i in range(ntiles + 4):
        for k_, st in stages:
            j = i - k_
            if 0 <= j < ntiles:
                st(j)
        yield


HD = 128
NH = 8
GW = 1024
TBLKS = [(0, 256)] + [(NCTX + i * 512, 512) for i in range(8)]


def declareB(G, nc, dbg=()):
    def inp(name, shape, dt=F32):
        return nc.dram_tensor(name, list(shape), dt, kind="ExternalInput").ap()
    def scr(name, shape, dt):
        kind = "ExternalOutput" if name in dbg else "Internal"
        return nc.dram_tensor(name, list(shape), dt, kind=kind).ap()
    G.conv_wT = inp("conv_wT", [128, 24, 5])
    G.ab_par = inp("ab_par", [1, 32])
    G.ident_h = inp("ident_h", [128, 128], F16)
    G.ones_f = inp("ones_f", [128, 128], F32)
    G.qT_d = scr("qT_d", [GW, NT], F16)
    G.kT_d = scr("kT_d", [GW, NT], F16)
    G.ktok_d = scr("ktok_d", [NT, GW], F16)
    G.vtok_d = scr("vtok_d", [NT, GW], F16)
    G.F_d = scr("F_d", [NX, GW], F16)
    G.zs_d = scr("zs_d", [NX, GW], F16)
    G.g_d = scr("g_d", [NT, 16], F32)
    G.beta_d = scr("beta_d", [NT, 16], F32)


def phase0A(G, S, es):
    g0 = phase0_mod(G, S, es)
    for _ in range(8):
        next(g0)
    gA = phaseA_modulate(G, S, es)
    n = 0
    done0 = False
    for _ in gA:
        n += 1
        if n % 2 == 0 and not done0:
            try:
                next(g0)
            except StopIteration:
                done0 = True
    for _ in g0:
        pass


def phaseB1_qkv(G, S, es):
    nc = G.nc
    NS = 8
    def T(name, shape, dt):
        return es.enter_context(nc.sbuf_tensor(name, shape, dt))
    wq = [T("wq%d" % i, [128, KC, 512], BF16) for i in range(2)]
    hTb = [T("hB%d" % i, [128, KC, 512], BF16) for i in range(3)]
    cw = T("cw", [128, 24, 5], F32)
    diagw = T("diagw", [128, 24, 5, 128], BF16)
    identh = T("identh", [128, 128], F16)
    onesb = T("onesb", [128, 128], BF16)
    pbx = [T("pbx%d" % i, [128, 8, 68], BF16) for i in range(NS)]
    pbc = [T("pbc%d" % i, [128, 1, 260], BF16) for i in range(2)]
    sl = [T("sl%d" % i, [128, 512], F32) for i in range(NS)]
    sq = [T("sq%d" % i, [128, 512], BF16) for i in range(NS)]
    rn = [T("rn%d" % i, [128, 512], F32) for i in range(NS)]
    o16 = [T("o16_%d" % i, [128, 512], F16) for i in range(NS)]
    tk = [T("tk%d" % i, [128, 4, 128], F16) for i in range(NS)]
    S.dma("sp", lambda e: e.dma_start(out=cw[:], in_=G.conv_wT), writes=["cw"])
    S.dma("sp", lambda e: e.dma_start(out=identh[:], in_=G.ident_h), writes=["identh"])
    S.op("pool", lambda e: e.memset(onesb[:], 1.0), writes=["onesb"])
    for i in range(NS):
        S.op("pool", lambda e, i=i: e.memset(pbx[i][:], 0.0), writes=[("pbx", i)])
    for i in range(2):
        S.op("pool", lambda e, i=i: e.memset(pbc[i][:], 0.0), writes=[("pbc", i)])
    for ch in range(24):
        for j in range(5):
            S.op("dve" if (ch + j) % 2 else "pool", lambda e, ch=ch, j=j: e.tensor_scalar(out=diagw[:, ch, j, :], in0=G.ident[:], scalar1=cw[:, ch, j:j + 1], scalar2=None, op0=ALU.mult),
                 reads=["cw", "ident"], writes=[("diagw", ch)])
    wv = G.w_in.rearrange("(k p) n -> p k n", p=128)
    hv = G.hT_d.rearrange("(k p) t -> p k t", p=128)
    psh = [G.ps[6 + i][:].bitcast(F16) for i in range(2)]
    pairs = []
    nb = 0
    nctx = 0
    for grp in range(6):
        kind = grp // 2
        for bi, (t0, bw) in enumerate(TBLKS):
            if kind == 0 and bi == 0:
                continue
            hs = nb % 3
            nb += 1
            for c4 in range(4):
                pairs.append(dict(grp=grp, kind=kind, bi=bi, t0=t0, bw=bw, hs=hs, c4=c4, first=(c4 == 0), chunk=grp * 4 + c4,
                                  head=(grp * 4 + c4) % 8, wfirst=(c4 == 0 and (bi == (1 if kind == 0 else 0)))))
    for n, p in enumerate(pairs):
        p["n"] = n
        p["s"] = n % NS
        if p["bi"] == 0:
            p["cs"] = nctx % 2
            nctx += 1

    def st_proj(p):
        ws = p["grp"] % 2; hs = p["hs"]; bw = p["bw"]; t0 = p["t0"]; c4 = p["c4"]; n = p["n"]
        b = n % 2
        for k in range(KC):
            S.op("pe", lambda e, ws=ws, k=k, c4=c4, hs=hs, b=b, bw=bw: e.matmul(
                G.ps[b][:, 0:bw], lhsT=wq[ws][:, k, c4 * 128:(c4 + 1) * 128], rhs=hTb[hs][:, k, 0:bw], start=(k == 0), stop=(k == KC - 1)),
                reads=[("wq", ws), ("hB", hs)], writes=[("ps", b)])
        if p["bi"] == 0:
            cs = p["cs"]
            S.op("act", lambda e, b=b, cs=cs: e.activation(out=pbc[cs][:, :, 2:258], in_=G.ps[b][:, 0:256].rearrange("p (r l) -> p r l", l=256), func=AF.Copy),
                 reads=[("ps", b)], writes=[("pbc", cs)])
        else:
            s = p["s"]
            S.op("act", lambda e, b=b, s=s: e.activation(out=pbx[s][:, :, 2:66], in_=G.ps[b][:, :].rearrange("p (r l) -> p r l", l=64), func=AF.Copy),
                 reads=[("ps", b)], writes=[("pbx", s)])

    def st_conv(p):
        n = p["n"]; s = p["s"]; bw = p["bw"]; chunk = p["chunk"]; kind = p["kind"]
        b = 2 + n % 2
        isc = p["bi"] == 0
        L = 256 if isc else 64
        src = pbc[p["cs"]] if isc else pbx[s]
        skey = ("pbc", p["cs"]) if isc else ("pbx", s)
        for j in range(5):
            S.op("pe", lambda e, b=b, bw=bw, chunk=chunk, j=j, src=src, L=L: e.matmul(
                G.ps[b][:, 0:bw].rearrange("p (r l) -> p r l", l=L), lhsT=diagw[:, chunk, j, :], rhs=src[:, :, j:j + L], start=(j == 0), stop=(j == 4)),
                reads=[skey, ("diagw", chunk)], writes=[("ps", b)])
        if kind == 2:
            S.op("act", lambda e, s=s, b=b, bw=bw: e.activation(out=o16[s][:, 0:bw], in_=G.ps[b][:, 0:bw], func=AF.Silu),
                 reads=[("ps", b)], writes=[("o16", s)])
        else:
            S.op("act", lambda e, s=s, b=b, bw=bw: e.activation(out=sl[s][:, 0:bw], in_=G.ps[b][:, 0:bw], func=AF.Silu),
                 reads=[("ps", b)], writes=[("sl", s)])
            S.op("pool", lambda e, s=s, bw=bw: e.tensor_tensor(out=sq[s][:, 0:bw], in0=sl[s][:, 0:bw], in1=sl[s][:, 0:bw], op=ALU.mult),
                 reads=[("sl", s)], writes=[("sq", s)])

    def st_norm(p):
        if p["kind"] == 2:
            return
        n = p["n"]
        if p.get("norm_done"):
            return
        group = [p]
        if n + 1 < len(pairs) and pairs[n + 1]["kind"] != 2 and n % 2 == 0:
            return
        if n % 2 == 1 and pairs[n - 1]["kind"] != 2 and not pairs[n - 1].get("norm_done"):
            group = [pairs[n - 1], p]
        for q in group:
            s = q["s"]; bw = q["bw"]; b = 4 + q["n"] % 2
            S.op("pe", lambda e, s=s, b=b, bw=bw: e.matmul(G.ps[b][:, 0:bw], lhsT=onesb[:], rhs=sq[s][:, 0:bw], start=True, stop=True),
                 reads=[("sq", s), "onesb"], writes=[("ps", b)])
        for q in group:
            s = q["s"]; bw = q["bw"]; b = 4 + q["n"] % 2
            S.op("act", lambda e, s=s, b=b, bw=bw: e.activation(out=rn[s][:, 0:bw], in_=G.ps[b][:, 0:bw], func=AF.Sqrt, bias=EPS, scale=1.0),
                 reads=[("ps", b)], writes=[("rn", s)])
        for q in group:
            s = q["s"]; bw = q["bw"]; kind = q["kind"]; head = q["head"]; t0 = q["t0"]
            S.op("dve", lambda e, s=s, bw=bw: e.reciprocal(out=rn[s][:, 0:bw], in_=rn[s][:, 0:bw]), reads=[("rn", s)], writes=[("rn", s)])
            qs = HD ** -0.5 if kind == 0 else 1.0
            S.op("dve", lambda e, s=s, bw=bw, qs=qs: e.scalar_tensor_tensor(out=o16[s][:, 0:bw], in0=sl[s][:, 0:bw], scalar=qs, in1=rn[s][:, 0:bw], op0=ALU.mult, op1=ALU.mult),
                 reads=[("sl", s), ("rn", s)], writes=[("o16", s)])
            dstT = G.qT_d if kind == 0 else G.kT_d
            S.dma("sp", lambda e, s=s, dstT=dstT, head=head, t0=t0, bw=bw: e.dma_start(out=dstT[head * 128:(head + 1) * 128, t0:t0 + bw], in_=o16[s][:, 0:bw]),
                  reads=[("o16", s)], writes=[("qkT_d", kind, head, q["bi"])])
            q["norm_done"] = True

    def st_tr(p):
        n = p["n"]; s = p["s"]; bw = p["bw"]; kind = p["kind"]; head = p["head"]; t0 = p["t0"]
        if kind == 0:
            return
        b = n % 2
        ntl = bw // 128
        for tt in range(ntl):
            S.op("pe", lambda e, s=s, tt=tt, b=b: e.transpose(out=psh[b][:, tt * 128:(tt + 1) * 128], in_=o16[s][:, tt * 128:(tt + 1) * 128], identity=identh[:]),
                 reads=[("o16", s), "identh"], writes=[("ps", 6 + b)])
        S.op("act", lambda e, s=s, b=b, ntl=ntl: e.activation(out=tk[s][:, 0:ntl, :], in_=psh[b][:, 0:ntl * 128].rearrange("p (a b) -> p a b", b=128), func=AF.Copy),
             reads=[("ps", 6 + b)], writes=[("tk", s)])
        dst = G.ktok_d if kind == 1 else G.vtok_d
        S.dma("sp", lambda e, s=s, dst=dst, head=head, t0=t0, bw=bw, ntl=ntl: e.dma_start(
            out=dst[t0:t0 + bw, head * 128:(head + 1) * 128].rearrange("(a p) c -> p a c", p=128), in_=tk[s][:, 0:ntl, :]),
            reads=[("tk", s)], writes=[("tok_d", kind, head, p["bi"])])

    def st_load(p):
        ws = p["grp"] % 2; hs = p["hs"]; bw = p["bw"]; t0 = p["t0"]
        if p["wfirst"]:
            c0 = 1024 + p["grp"] * 512
            S.dma("pool", lambda e, ws=ws, c0=c0: e.dma_start(out=wq[ws][:], in_=wv[:, :, c0:c0 + 512]), writes=[("wq", ws)])
        if p["first"]:
            S.dma("sp", lambda e, hs=hs, t0=t0, bw=bw: e.dma_start(out=hTb[hs][:, :, 0:bw], in_=hv[:, :, t0:t0 + bw]),
                  reads=[("hT_d", p["bi"])], writes=[("hB", hs)])

    PF = 7
    for j in range(min(PF, len(pairs))):
        st_load(pairs[j])
    stages = [(0, st_proj), (1, st_conv), (3, st_norm), (6, st_tr)]
    for i in range(len(pairs) + 6):
        if i + PF < len(pairs):
            st_load(pairs[i + PF])
        for k, st in stages:
            j = i - k
            if 0 <= j < len(pairs):
                st(pairs[j])


def phaseB2_fz(G, S, es):
    nc = G.nc
    wf = es.enter_context(nc.sbuf_tensor("wf", [128, KC, 2080], BF16))
    hTb = [es.enter_context(nc.sbuf_tensor("hC%d" % i, [128, KC, 512], BF16)) for i in range(2)]
    par = es.enter_context(nc.sbuf_tensor("par", [128, 32], F32))
    ea = es.enter_context(nc.sbuf_tensor("ea", [128, 16], F32))
    o16 = [es.enter_context(nc.sbuf_tensor("fo%d" % i, [128, 2048], F16)) for i in range(2)]
    ab = [es.enter_context(nc.sbuf_tensor("ab%d" % i, [128, 32], F32)) for i in range(2)]
    gb = [es.enter_context(nc.sbuf_tensor("gb%d" % i, [128, 32], F32)) for i in range(2)]
    wv = G.w_in.rearrange("(k p) n -> p k n", p=128)
    hv = G.hT_d.rearrange("(k p) t -> p k t", p=128)
    for i in range(4):
        c0 = (0, 512, 4096, 4608)[i]
        S.dma("pool", lambda e, i=i, c0=c0: e.dma_start(out=wf[:, :, i * 512:(i + 1) * 512], in_=wv[:, :, c0:c0 + 512]), writes=[("wf", i)])
    S.dma("pool", lambda e: e.dma_start(out=wf[:, :, 2048:2080], in_=wv[:, :, 5120:5152]), writes=[("wf", 4)])
    S.dma("sp", lambda e: e.dma_start(out=par[:], in_=bc(G.ab_par[0:1, :], 32)), writes=["par"])
    S.op("act", lambda e: e.activation(out=ea[:], in_=par[:, 0:16], func=AF.Exp), reads=["par"], writes=["ea"])
    S.op("dve", lambda e: e.tensor_scalar(out=ea[:], in0=ea[:], scalar1=-1.0, scalar2=None, op0=ALU.mult), reads=["ea"], writes=["ea"])
    n = 0
    for bi, (t0, bw) in enumerate(TBLKS):
        hs = bi % 2
        S.dma("sp", lambda e, hs=hs, t0=t0, bw=bw: e.dma_start(out=hTb[hs][:, :, 0:bw], in_=hv[:, :, t0:t0 + bw]),
              reads=[("hT_d", bi)], writes=[("hC", hs)])
        for tt in range(bw // 128):
            s = n % 2
            n += 1
            tok = t0 + tt * 128
            pb = G.ps[4 + s]
            for k in range(KC):
                S.op("pe", lambda e, k=k, hs=hs, tt=tt, pb=pb: e.matmul(pb[:, 0:32], lhsT=hTb[hs][:, k, tt * 128:(tt + 1) * 128],
                                                                     rhs=wf[:, k, 2048:2080], start=(k == 0), stop=(k == KC - 1)),
                     reads=[("hC", hs), ("wf", 4)], writes=[("ps", 4 + s)])
            S.op("dve", lambda e, s=s, pb=pb: e.tensor_tensor(out=ab[s][:, 0:16], in0=pb[:, 0:16], in1=par[:, 16:32], op=ALU.add),
                 reads=[("ps", 4 + s), "par"], writes=[("ab", s)])
            S.op("act", lambda e, s=s: e.activation(out=ab[s][:, 0:16], in_=ab[s][:, 0:16], func=AF.Exp), reads=[("ab", s)], writes=[("ab", s)])
            S.op("act", lambda e, s=s: e.activation(out=ab[s][:, 0:16], in_=ab[s][:, 0:16], func=AF.Ln, bias=1.0, scale=1.0), reads=[("ab", s)], writes=[("ab", s)])
            S.op("dve", lambda e, s=s: e.tensor_tensor(out=gb[s][:, 0:16], in0=ab[s][:, 0:16], in1=ea[:], op=ALU.mult),
                 reads=[("ab", s), "ea"], writes=[("gb", s)])
            S.op("act", lambda e, s=s, pb=pb: e.activation(out=ab[s][:, 16:32], in_=pb[:, 16:32], func=AF.Exp, scale=-1.0),
                 reads=[("ps", 4 + s)], writes=[("ab2", s)])
            S.op("dve", lambda e, s=s: e.tensor_scalar(out=ab[s][:, 16:32], in0=ab[s][:, 16:32], scalar1=1.0, scalar2=None, op0=ALU.add),
                 reads=[("ab2", s)], writes=[("ab2", s)])
            S.op("dve", lambda e, s=s: e.reciprocal(out=gb[s][:, 16:32], in_=ab[s][:, 16:32]), reads=[("ab2", s)], writes=[("gb2", s)])
            S.dma("act", lambda e, s=s, tok=tok: e.dma_start(out=G.g_d[tok:tok + 128, :], in_=gb[s][:, 0:16]), reads=[("gb", s)], writes=[("g_d", tok)])
            S.dma("act", lambda e, s=s, tok=tok: e.dma_start(out=G.beta_d[tok:tok + 128, :], in_=gb[s][:, 16:32]), reads=[("gb2", s)], writes=[("beta_d", tok)])
            if bi == 0:
                continue
            for cb in range(4):
                pb2 = G.ps[cb]
                for k in range(KC):
                    S.op("pe", lambda e, k=k, hs=hs, tt=tt, pb2=pb2, cb=cb: e.matmul(
                        pb2[:, :], lhsT=hTb[hs][:, k, tt * 128:(tt + 1) * 128], rhs=wf[:, k, cb * 512:(cb + 1) * 512],
                        start=(k == 0), stop=(k == KC - 1)),
                        reads=[("hC", hs), ("wf", cb)], writes=[("ps", cb)])
                if cb < 2:
                    S.op("dve", lambda e, s=s, cb=cb, pb2=pb2: e.tensor_copy(out=o16[s][:, cb * 512:(cb + 1) * 512], in_=pb2[:, :]),
                         reads=[("ps", cb)], writes=[("fo", s, cb)])
                else:
                    S.op("act", lambda e, s=s, cb=cb, pb2=pb2: e.activation(out=o16[s][:, cb * 512:(cb + 1) * 512], in_=pb2[:, :], func=AF.Silu),
                         reads=[("ps", cb)], writes=[("fo", s, cb)])
            xt0 = tok - NCTX
            S.dma("act", lambda e, s=s, xt0=xt0: e.dma_start(out=G.F_d[xt0:xt0 + 128, :], in_=o16[s][:, 0:1024]),
                  reads=[("fo", s, 0), ("fo", s, 1)], writes=[("F_d", xt0)])
            S.dma("act", lambda e, s=s, xt0=xt0: e.dma_start(out=G.zs_d[xt0:xt0 + 128, :], in_=o16[s][:, 1024:2048]),
                  reads=[("fo", s, 2), ("fo", s, 3)], writes=[("zs_d", xt0)])


def declareG(G, nc, dbg=()):
    def inp(name, shape, dt=F32):
        return nc.dram_tensor(name, list(shape), dt, kind="ExternalInput").ap()
    def scr(name, shape, dt):
        kind = "ExternalOutput" if name in dbg else "Internal"
        return nc.dram_tensor(name, list(shape), dt, kind=kind).ap()
    G.cmask = inp("cmask", [128, 2, 6, 128])
    G.o_d = [scr("o_d%d" % d, [NX, GW], F32) for d in range(2)]


def gdn_masks():
    i = np.arange(128)[:, None]; j = np.arange(128)[None, :]
    m = np.zeros((128, 2, 6, 128), np.float32)
    bi, bj = i // 32, j // 32
    m[:, 0, 0] = (i <= j)
    m[:, 1, 0] = (i >= j)
    m[:, 0, 1] = (i > j)
    m[:, 1, 1] = (i < j)
    m[:, 0, 2] = (i > j) & (bi == bj)
    m[:, 1, 2] = (i < j) & (bi == bj)
    m[:, 0, 3] = (bi % 2 == 1) & (bj == bi - 1)
    m[:, 1, 3] = (bi % 2 == 0) & (bj == bi + 1)
    m[:, 0, 4] = (i >= 64) & (j < 64)
    m[:, 1, 4] = (i < 64) & (j >= 64)
    m[:, 0, 5] = (j >= i)
    m[:, 1, 5] = (j <= i)
    return m


def phaseG_gdn(G, S, es):
    nc = G.nc
    import os
    def T(name, shape, dt):
        return es.enter_context(nc.sbuf_tensor(name, shape, dt))
    cm = T("cm", [128, 2, 6, 128], F32)
    identh = T("identh2", [128, 128], F16)
    onesf = T("onesf2", [128, 128], F32)
    eye32 = T("eye32", [128, 128], F32)
    S.dma("sp", lambda e: e.dma_start(out=cm[:], in_=G.cmask), writes=["cm"])
    S.dma("sp", lambda e: e.dma_start(out=identh[:], in_=G.ident_h), writes=["identh2"])
    S.dma("sp", lambda e: e.dma_start(out=onesf[:], in_=G.ones_f), writes=["onesf2"])
    S.op("act", lambda e: e.activation(out=eye32[:], in_=identh[:], func=AF.Copy), reads=["identh2"], writes=["eye32"])
    psh = [G.ps[b][:].bitcast(F16) for b in range(8)]
    state = {"bank": 0}

    def banks():
        b = state["bank"]
        state["bank"] = (b + 1) % 8
        return b, b

    def bch(ap2):
        return bass.AP(ap2.tensor, ap2.offset, [list(ap2.ap[0]), [0, NHS], list(ap2.ap[1])])

    def bcj(ap2, n=NH):
        return bass.AP(ap2.tensor, ap2.offset, [list(ap2.ap[0]), list(ap2.ap[1]), [0, 128]])

    def v32(b):
        return G.ps[b][:, :].rearrange("p (q n) -> p q n", q=4)

    def v16(b):
        return psh[b][:, :].rearrange("p (q n) -> p q n", q=4)[:, :, 0:128]

    NHS = 4
    def make_dir(d, hg):
        X = "d%d_%d_" % (d, hg)
        def H(name, dt):
            return T(X + name, [128, NHS, 128], dt)
        kT2 = [T(X + "kT%d" % i, [128, NHS, 128], F16) for i in range(2)]; qT2 = [T(X + "qT%d" % i, [128, NHS, 128], F16) for i in range(2)]
        kt2 = [T(X + "kt%d" % i, [128, NHS, 128], F16) for i in range(2)]; vt2 = [T(X + "vt%d" % i, [128, NHS, 128], F16) for i in range(2)]
        gu2 = [T(X + "gu%d" % i, [128, 16], F32) for i in range(2)]; bu2 = [T(X + "bu%d" % i, [128, 16], F32) for i in range(2)]
        decb = H("decb", F32)
        egc = T(X + "egc", [128, NHS], F32); erem = T(X + "erem", [128, NHS], F32); etot = T(X + "etot", [128, NHS], F32)
        gcs = T(X + "gcs", [128, NHS], F32); bsc = T(X + "bsc", [128, NHS], F32)
        S32 = H("S32", F32); S16 = H("S16", F16)
        G2 = H("G2", F32); dec = H("dec", F32); decT = H("decT", F32)
        P = [H("Pa", F16), H("Pb", F16)]; PT = [H("PTa", F16), H("PTb", F16)]
        Mo1 = H("Mo1", F16); Mo1T = H("Mo1T", F16); Mo2 = H("Mo2", F16)
        Rr = [H("Ra", F16), H("Rb", F16)]; Tt = [H("Ta", F16), H("Tb", F16)]
        Z = H("Z", F16); Z2 = H("Z2", F16)
        qkm = H("qkm", F16); kb = H("kb", F16); vb = H("vb", F16); kdc = H("kdc", F16)
        uu = H("uu", F32); wT = H("wT", F16); vnew = H("vnew", F16); otmp = H("otmp", F32); oo = H("oo", F32)
        K0 = lambda n: (X + n)
        K = K0
        ALLG = (0, 1)

        def smm(fl, fr, rd):
            b0, b1 = banks()
            for h in range(NHS):
                b = b0
                q = h
                l_ = fl(h); r_ = fr(h)
                S.op("pe", lambda e, b=b, q=q, l_=l_, r_=r_: e.matmul(G.ps[b][:, q * 128:(q + 1) * 128], lhsT=l_, rhs=r_, start=True, stop=True),
                     reads=rd, writes=[("ps", b)])
            return (b0,)

        def strp(src, rd):
            b0, b1 = banks()
            for h in range(NHS):
                b = b0
                q = h
                S.op("pe", lambda e, b=b, q=q, h=h: e.transpose(out=psh[b][:, q * 256:q * 256 + 128], in_=src[:, h, :], identity=identh[:]),
                     reads=rd + ["identh2"], writes=[("ps", b)])
            return (b0,)

        def evac(eng, bb, dst, dkey, f16=False, fn=None, extra=()):
            for g, b in enumerate(bb):
                src = v16(b) if f16 else v32(b)
                o = dst[:, 4 * g:4 * g + 4, :]
                if fn is None:
                    if eng == "act":
                        S.op("act", lambda e, o=o, src=src: e.activation(out=o, in_=src, func=AF.Copy), reads=[("ps", b)] + list(extra), writes=[dkey])
                    else:
                        S.op("dve", lambda e, o=o, src=src: e.tensor_copy(out=o, in_=src), reads=[("ps", b)] + list(extra), writes=[dkey])
                else:
                    S.op(eng, (lambda e, o=o, src=src, g=g: fn(e, o, src, g)), reads=[("ps", b)] + list(extra), writes=[dkey])

        S.op("pool", lambda e: e.memset(S32[:], 0.0), writes=[K("S32")])
        S.op("pool", lambda e: e.memset(S16[:], 0.0), writes=[K("S16")])
        nunits = NT // 128
        order = list(range(nunits)) if d == 0 else [1, 0] + list(range(nunits - 1, 1, -1))
        order = order[:int(os.environ.get("GUNITS", "99"))]
        c8 = slice(d * 8 + hg * 4, d * 8 + hg * 4 + 4)
        hr = slice(hg * 512, (hg + 1) * 512)
        A1 = cm[:, d, 0, :]; B1 = cm[:, d, 1, :]

        def loads(u, sl_):
            tok = u * 128
            kT, qT, kt, vt, gu, bu = kT2[sl_], qT2[sl_], kt2[sl_], vt2[sl_], gu2[sl_], bu2[sl_]
            ks = str(sl_)
            S.dma("sp", lambda e: e.dma_start(out=kT[:], in_=G.kT_d[hr, tok:tok + 128].rearrange("(h p) t -> p h t", p=128)),
                  reads=[("qkT_d", 1, h, b_) for h in range(NH) for b_ in range(9)], writes=[K("kT" + ks)])
            if u >= 2:
                S.dma("sp", lambda e: e.dma_start(out=qT[:], in_=G.qT_d[hr, tok:tok + 128].rearrange("(h p) t -> p h t", p=128)),
                      reads=[("qkT_d", 0, h, b_) for h in range(NH) for b_ in range(1, 9)], writes=[K("qT" + ks)])
            S.dma("sp", lambda e: e.dma_start(out=kt[:].rearrange("p h c -> p (h c)"), in_=G.ktok_d[tok:tok + 128, hr]),
                  reads=[("tok_d", 1, h, b_) for h in range(NH) for b_ in range(9)], writes=[K("kt" + ks)])
            S.dma("sp", lambda e: e.dma_start(out=vt[:].rearrange("p h c -> p (h c)"), in_=G.vtok_d[tok:tok + 128, hr]),
                  reads=[("tok_d", 2, h, b_) for h in range(NH) for b_ in range(9)], writes=[K("vt" + ks)])
            S.dma("sp", lambda e: e.dma_start(out=gu[:], in_=G.g_d[tok:tok + 128, :]), reads=[("g_d", tok)], writes=[K("gu" + ks)])
            S.dma("sp", lambda e: e.dma_start(out=bu[:], in_=G.beta_d[tok:tok + 128, :]), reads=[("beta_d", tok)], writes=[K("bu" + ks)])

        def unit(u, ui, unext):
            tok = u * 128
            isx = u >= 2
            sl_ = ui % 2
            kT, qT, kt, vt, gu, bu = kT2[sl_], qT2[sl_], kt2[sl_], vt2[sl_], gu2[sl_], bu2[sl_]
            def K(n, sl_=sl_):
                return K0(n + str(sl_)) if n in ("kT", "qT", "kt", "vt", "gu", "bu") else K0(n)
            if ui == 0:
                loads(u, 0)
            if unext is not None:
                loads(unext, 1 - sl_)
            b0, _ = banks()
            S.op("pe", lambda e: e.matmul(G.ps[b0][:, 0:4], lhsT=A1, rhs=gu[:, c8], start=True, stop=True), reads=["cm", K("gu")], writes=[("ps", b0)])
            S.op("pe", lambda e: e.matmul(G.ps[b0][:, 8:12], lhsT=onesf[:], rhs=gu[:, c8], start=True, stop=True), reads=["onesf2", K("gu")], writes=[("ps", b0)])
            S.op("act", lambda e: e.activation(out=egc[:], in_=G.ps[b0][:, 0:4], func=AF.Exp), reads=[("ps", b0)], writes=[K("egc")])
            S.op("act", lambda e: e.activation(out=etot[:], in_=G.ps[b0][:, 8:12], func=AF.Exp), reads=[("ps", b0)], writes=[K("etot")])
            S.op("act", lambda e: e.activation(out=gcs[:], in_=G.ps[b0][:, 0:4], func=AF.Copy), reads=[("ps", b0)], writes=[K("gcs")])
            S.op("dve", lambda e: e.tensor_tensor(out=erem[:], in0=G.ps[b0][:, 8:12], in1=gcs[:], op=ALU.subtract), reads=[("ps", b0), K("gcs")], writes=[K("erem")])
            S.op("act", lambda e: e.activation(out=erem[:], in_=erem[:], func=AF.Exp), reads=[K("erem")], writes=[K("erem")])
            S.op("dve", lambda e: e.tensor_tensor(out=bsc[:], in0=bu[:, c8], in1=egc[:], op=ALU.mult), reads=[K("bu"), K("egc")], writes=[K("bsc")])
            S.op("pool", lambda e: e.tensor_tensor(out=G2[:], in0=bch(B1), in1=bcj(gu[:, c8]), op=ALU.mult), reads=["cm", K("gu")], writes=[K("G2")])
            yield
            bb = smm(lambda h: A1, lambda h: G2[:, h, :], ["cm", K("G2")])
            evac("act", bb, dec, K("dec"), fn=lambda e, o, src, g: e.activation(out=o, in_=src, func=AF.Exp))
            S.op("pool", lambda e: e.tensor_tensor(out=dec[:], in0=dec[:], in1=bcj(bu[:, c8]), op=ALU.mult), reads=[K("dec"), K("bu")], writes=[K("dec")])
            S.op("pool", lambda e: e.tensor_tensor(out=decb[:], in0=dec[:], in1=bch(cm[:, d, 2, :]), op=ALU.mult), reads=[K("dec"), "cm"], writes=[K("decb")])
            yield
            bb = smm(lambda h: G2[:, h, :], lambda h: A1, ["cm", K("G2")])
            evac("act", bb, decT, K("decT"), fn=lambda e, o, src, g: e.activation(out=o, in_=src, func=AF.Exp))
            S.op("pool", lambda e: e.tensor_tensor(out=decT[:], in0=decT[:], in1=bch(cm[:, d, 5, :]), op=ALU.mult), reads=[K("decT"), "cm"], writes=[K("decT")])
            yield
            bb = smm(lambda h: kT[:, h, :], lambda h: kT[:, h, :], [K("kT")])
            evac("dve", bb, P[0], K("P0"), fn=lambda e, o, src, g: e.tensor_tensor(out=o, in0=src, in1=decb[:], op=ALU.mult), extra=[K("decb")])
            evac("dve", bb, G2, K("G2"), fn=lambda e, o, src, g: e.tensor_tensor(out=o, in0=src, in1=dec[:], op=ALU.mult), extra=[K("dec")])
            S.op("pool", lambda e: e.tensor_tensor(out=Mo1[:], in0=G2[:], in1=bch(cm[:, d, 3, :]), op=ALU.mult), reads=[K("G2"), "cm"], writes=[K("Mo1")])
            S.op("pool", lambda e: e.tensor_tensor(out=Mo2[:], in0=G2[:], in1=bch(cm[:, d, 4, :]), op=ALU.mult), reads=[K("G2"), "cm"], writes=[K("Mo2")])
            S.op("dve", lambda e: e.tensor_tensor(out=kb[:], in0=kt[:], in1=bcj(bsc[:]), op=ALU.mult), reads=[K("kt"), K("bsc")], writes=[K("kb")])
            S.op("dve", lambda e: e.tensor_tensor(out=vb[:], in0=vt[:], in1=bcj(bu[:, c8]), op=ALU.mult), reads=[K("vt"), K("bu")], writes=[K("vb")])
            S.op("pool", lambda e: e.tensor_tensor(out=kdc[:], in0=kt[:], in1=bcj(erem[:]), op=ALU.mult), reads=[K("kt"), K("erem")], writes=[K("kdc")])
            yield
            if isx:
                bb = smm(lambda h: kT[:, h, :], lambda h: qT[:, h, :], [K("kT"), K("qT")])
                evac("dve", bb, qkm, K("qkm"), fn=lambda e, o, src, g: e.tensor_tensor(out=o, in0=src, in1=decT[:, 4 * g:4 * g + 4, :], op=ALU.mult), extra=[K("decT")])
                yield
            bb = strp(P[0], [K("P0")])
            evac("act", bb, PT[0], K("PT0"), f16=True)
            yield
            S.op("pool", lambda e: e.tensor_tensor(out=Rr[0][:], in0=bch(eye32[:]), in1=PT[0][:], op=ALU.subtract), reads=["eye32", K("PT0")], writes=[K("R0")])
            yield
            cur = 0
            for lvl in range(4):
                nxt = 1 - cur
                Pc, PTc, Pn, PTn, Rc, Rn, Tc, Tn = P[cur], PT[cur], P[nxt], PT[nxt], Rr[cur], Rr[nxt], Tt[cur], Tt[nxt]
                kc_, kn_ = str(cur), str(nxt)
                bb = smm(lambda h: PTc[:, h, :], lambda h: Pc[:, h, :], [K("PT" + kc_), K("P" + kc_)])
                evac("act", bb, Pn, K("P" + kn_))
                yield
                if lvl < 3:
                    bb = smm(lambda h: Pc[:, h, :], lambda h: PTc[:, h, :], [K("PT" + kc_), K("P" + kc_)])
                    evac("act", bb, PTn, K("PT" + kn_))
                    yield
                bb = smm(lambda h: Pn[:, h, :], lambda h: Rc[:, h, :], [K("P" + kn_), K("R" + kc_)])
                evac("dve", bb, Rn, K("R" + kn_), fn=lambda e, o, src, g, Rc=Rc: e.tensor_tensor(out=o, in0=Rc[:, 4 * g:4 * g + 4, :], in1=src, op=ALU.add), extra=[K("R" + kc_)])
                yield
                cur = nxt
            nxt = 1 - cur
            Rc, Rn = Rr[cur], Rr[nxt]
            kc_, kn_ = str(cur), str(nxt)
            bb = strp(Rc, [K("R" + kc_)])
            evac("act", bb, Tt[0], K("TT0"), f16=True)
            yield
            bb = smm(lambda h: Mo1[:, h, :], lambda h: Rc[:, h, :], [K("Mo1"), K("R" + kc_)])
            evac("act", bb, Z, K("Z"))
            yield
            bb = smm(lambda h: Tt[0][:, h, :], lambda h: Z[:, h, :], [K("TT0"), K("Z")])
            evac("dve", bb, Rn, K("R" + kn_), fn=lambda e, o, src, g, Rc=Rc: e.tensor_tensor(out=o, in0=Rc[:, 4 * g:4 * g + 4, :], in1=src, op=ALU.subtract), extra=[K("R" + kc_)])
            yield
            cur = nxt
            nxt = 1 - cur
            Rc, Rn = Rr[cur], Rr[nxt]
            kc_, kn_ = str(cur), str(nxt)
            bb = strp(Rc, [K("R" + kc_)])
            evac("act", bb, Tt[1], K("TT1"), f16=True)
            yield
            bb = smm(lambda h: Mo2[:, h, :], lambda h: Rc[:, h, :], [K("Mo2"), K("R" + kc_)])
            evac("act", bb, Z, K("Z"))
            yield
            bb = smm(lambda h: Tt[1][:, h, :], lambda h: Z[:, h, :], [K("TT1"), K("Z")])
            evac("dve", bb, Rn, K("R" + kn_), fn=lambda e, o, src, g, Rc=Rc: e.tensor_tensor(out=o, in0=Rc[:, 4 * g:4 * g + 4, :], in1=src, op=ALU.subtract), extra=[K("R" + kc_)])
            yield
            Rf = Rn; rk = K("R" + kn_)
            bb = smm(lambda h: Rf[:, h, :], lambda h: vb[:, h, :], [rk, K("vb")])
            evac("act", bb, uu, K("uu"))
            yield
            bb = smm(lambda h: kb[:, h, :], lambda h: Rf[:, h, :], [rk, K("kb")])
            evac("act", bb, wT, K("wT"))
            yield
            bb = smm(lambda h: wT[:, h, :], lambda h: S16[:, h, :], [K("wT"), K("S16")])
            evac("dve", bb, vnew, K("vnew"), fn=lambda e, o, src, g: e.tensor_tensor(out=o, in0=uu[:, 4 * g:4 * g + 4, :], in1=src, op=ALU.subtract), extra=[K("uu")])
            yield
            if isx:
                bb = smm(lambda h: qT[:, h, :], lambda h: S16[:, h, :], [K("qT"), K("S16")])
                evac("dve", bb, otmp, K("otmp"), fn=lambda e, o, src, g: e.tensor_tensor(out=o, in0=src, in1=bcj(egc[:, 4 * g:4 * g + 4]), op=ALU.mult), extra=[K("egc")])
                yield
                bb = smm(lambda h: qkm[:, h, :], lambda h: vnew[:, h, :], [K("qkm"), K("vnew")])
                evac("dve", bb, oo, K("oo"), fn=lambda e, o, src, g: e.tensor_tensor(out=o, in0=otmp[:, 4 * g:4 * g + 4, :], in1=src, op=ALU.add), extra=[K("otmp")])
                xt0 = tok - NCTX
                S.dma("sp", lambda e: e.dma_start(out=G.o_d[d][xt0:xt0 + 128, hr], in_=oo[:].rearrange("p h v -> p (h v)")),
                      reads=[K("oo")], writes=[("o_d", d, xt0, hg)])
                yield
            bb = smm(lambda h: kdc[:, h, :], lambda h: vnew[:, h, :], [K("kdc"), K("vnew")])
            S.op("pool", lambda e: e.tensor_tensor(out=S32[:], in0=S32[:], in1=bcj(etot[:]), op=ALU.mult), reads=[K("S32"), K("etot")], writes=[K("S32")])
            evac("dve", bb, S32, K("S32"), fn=lambda e, o, src, g: e.tensor_tensor(out=o, in0=S32[:, 4 * g:4 * g + 4, :], in1=src, op=ALU.add), extra=[K("S32")])
            S.op("act", lambda e: e.activation(out=S16[:], in_=S32[:], func=AF.Copy), reads=[K("S32")], writes=[K("S16")])
            yield

        def run():
            for ui, u in enumerate(order):
                yield from unit(u, ui, order[ui + 1] if ui + 1 < len(order) else None)
        return run()

    gens = [make_dir(0, 0), make_dir(1, 0), make_dir(0, 1), make_dir(1, 1)]
    alive = [True] * 4
    while any(alive):
        for i, g in enumerate(gens):
            if alive[i]:
                try:
                    next(g)
                except StopIteration:
                    alive[i] = False


def declareF(G, nc, dbg=()):
    def inp(name, shape, dt=F32):
        return nc.dram_tensor(name, list(shape), dt, kind="ExternalInput").ap()
    def scr(name, shape, dt):
        kind = "ExternalOutput" if name in dbg else "Internal"
        return nc.dram_tensor(name, list(shape), dt, kind=kind).ap()
    G.W1 = inp("W1", [64, 128], F16)
    G.W2 = inp("W2", [128, 2, 256], F16)
    G.W3 = inp("W3", [64, 64, 2, 64], F16)
    G.gnw = inp("gnw", [1, 128])
    G.mixT_d = scr("mixT_d", [D, NX], BF16)


def fourier_tables():
    a = np.arange(64)
    ang = 2 * np.pi * np.outer(a, a) / 64
    W1 = np.concatenate([np.cos(ang), -np.sin(ang)], 1)
    c = np.arange(128)
    ang2 = 2 * np.pi * np.outer(c, c) / 128
    W2 = np.stack([np.concatenate([np.cos(ang2), -np.sin(ang2)], 1),
                   np.concatenate([np.sin(ang2), np.cos(ang2)], 1)], 1)
    b = np.arange(64)[:, None, None]; o2 = np.arange(64)[None, :, None]; o1 = np.arange(64)[None, None, :]
    th = 2 * np.pi * (b * o2 / 4096.0 + b * o1 / 64.0)
    sc = 1.0 / np.sqrt(4096 * 128)
    W3 = np.stack([np.cos(th) * sc, np.sin(th) * sc], 2)
    return W1.astype(np.float16), W2.astype(np.float16), W3.astype(np.float16)


def phaseF_fourier(G, S, es):
    nc = G.nc
    def T(name, shape, dt):
        return es.enter_context(nc.sbuf_tensor(name, shape, dt))
    w1 = T("w1", [64, 128], F16); w2 = T("w2", [128, 2, 256], F16); w3 = T("w3", [64, 64, 2, 64], F16)
    S.dma("sp", lambda e: e.dma_start(out=w1[:], in_=G.W1), writes=["w1"])
    S.dma("sp", lambda e: e.dma_start(out=w2[:], in_=G.W2), writes=["w2"])
    S.dma("sp", lambda e: e.dma_start(out=w3[:], in_=G.W3), writes=["w3"])
    Fg = [T("Fg%d" % i, [64, 64, 256], F16) for i in range(2)]
    P1 = T("P1", [128, 64, 128], F16)
    J = T("J", [64, 64, 256], F16)
    YT = [T("YT%d" % i, [128, NX], BF16) for i in range(2)]
    Fv = G.F_d.rearrange("(a b) c -> a b c", b=64)
    nb = 0
    def nextbank():
        nonlocal nb
        b = nb % 8
        nb += 1
        return b
    ev = 0
    for gp in range(4):
        fs = gp % 2
        S.dma("sp", lambda e, fs=fs, gp=gp: e.dma_start(out=Fg[fs][:], in_=Fv[:, :, gp * 256:(gp + 1) * 256]),
              reads=[("F_d", t * 128) for t in range(32)], writes=[("Fg", fs)])
        for gi in range(2):
            g = gp * 2 + gi
            ys = g % 2
            for b4 in range(16):
                bk = nextbank()
                for q in range(4):
                    b = b4 * 4 + q
                    S.op("pe", lambda e, fs=fs, gi=gi, b=b, bk=bk, q=q: e.matmul(G.ps[bk][:, q * 128:(q + 1) * 128],
                         lhsT=Fg[fs][:, b, gi * 128:(gi + 1) * 128], rhs=w1[:], start=True, stop=True),
                         reads=[("Fg", fs), "w1"], writes=[("psb", bk)])
                eng = "act" if ev % 2 == 0 else "dve"; ev += 1
                def f(e, bk=bk, b4=b4, eng=eng):
                    o = P1[:, b4 * 4:(b4 + 1) * 4, :]
                    i = G.ps[bk][:, :].rearrange("p (q n) -> p q n", q=4)
                    return e.activation(out=o, in_=i, func=AF.Copy) if eng == "act" else e.tensor_copy(out=o, in_=i)
                S.op(eng, f, reads=[("psb", bk)], writes=[("P1", b4)])
            for o22 in range(32):
                bk = nextbank()
                for q in range(2):
                    o2 = o22 * 2 + q
                    for ri in range(2):
                        S.op("pe", lambda e, o2=o2, ri=ri, bk=bk, q=q: e.matmul(G.ps[bk][0:64, q * 256:(q + 1) * 256],
                             lhsT=P1[:, :, ri * 64 + o2], rhs=w2[:, ri, :], start=(ri == 0), stop=(ri == 1)),
                             reads=[("P1", x_) for x_ in range(16)] + ["w2"], writes=[("psb", bk)])
                eng = "act" if ev % 2 == 0 else "dve"; ev += 1
                def f(e, bk=bk, o22=o22, eng=eng):
                    o = J[:, o22 * 2:(o22 + 1) * 2, :]
                    i = G.ps[bk][0:64, :].rearrange("p (q n) -> p q n", q=2)
                    return e.activation(out=o, in_=i, func=AF.Copy) if eng == "act" else e.tensor_copy(out=o, in_=i)
                S.op(eng, f, reads=[("psb", bk)], writes=[("J", o22)])
            for o28 in range(8):
                bk = nextbank()
                for q in range(8):
                    o2 = o28 * 8 + q
                    for ri in range(2):
                        S.op("pe", lambda e, o2=o2, ri=ri, bk=bk, q=q: e.matmul(G.ps[bk][:, q * 64:(q + 1) * 64],
                             lhsT=J[:, o2, ri * 128:(ri + 1) * 128], rhs=w3[:, o2, ri, :], start=(ri == 0), stop=(ri == 1)),
                             reads=[("J", x_) for x_ in range(32)] + ["w3"], writes=[("psb", bk)])
                eng = "act" if ev % 2 == 0 else "dve"; ev += 1
                def f(e, bk=bk, o28=o28, eng=eng, ys=ys):
                    o = YT[ys][:, :].rearrange("p (o1 o2) -> p o2 o1", o2=64)[:, o28 * 8:(o28 + 1) * 8, :]
                    i = G.ps[bk][:, :].rearrange("p (q n) -> p q n", q=8)
                    return e.activation(out=o, in_=i, func=AF.Copy) if eng == "act" else e.tensor_copy(out=o, in_=i)
                S.op(eng, f, reads=[("psb", bk)], writes=[("YT", ys, o28)])
            S.dma("act", lambda e, ys=ys, g=g: e.dma_start(out=G.mixT_d[g * 128:(g + 1) * 128, :], in_=YT[ys][:]),
                  reads=[("YT", ys, x_) for x_ in range(8)], writes=[("mixT_d", g)])


def phaseO_gdnout(G, S, es):
    nc = G.nc
    def T(name, shape, dt):
        return es.enter_context(nc.sbuf_tensor(name, shape, dt))
    gn = T("gn", [128, 128], F32)
    S.dma("sp", lambda e: e.dma_start(out=gn[:], in_=bc(G.gnw[0:1, :], 128)), writes=["gn"])
    of = [T("of%d" % i, [128, GW], F32) for i in range(4)]
    ob = [T("ob%d" % i, [128, GW], F32) for i in range(4)]
    zt = [T("zt%d" % i, [128, GW], F16) for i in range(4)]
    sqt = T("sqt", [128, GW], F32)
    ms = [T("ms%d" % i, [128, NH], F32) for i in range(2)]
    on = [T("on%d" % i, [128, GW], BF16) for i in range(2)]
    oT = [T("oT%d" % i, [128, NH, 128], BF16) for i in range(2)]
    ident = G.ident

    def o_load(t):
        s4 = t % 4; r0 = t * 128
        S.dma("sp", lambda e: e.dma_start(out=of[s4][:], in_=G.o_d[0][r0:r0 + 128, :]), reads=[("o_d", 0, r0, 0), ("o_d", 0, r0, 1)], writes=[("of", s4)])
        S.dma("sp", lambda e: e.dma_start(out=ob[s4][:], in_=G.o_d[1][r0:r0 + 128, :]), reads=[("o_d", 1, r0, 0), ("o_d", 1, r0, 1)], writes=[("ob", s4)])
        S.dma("sp", lambda e: e.dma_start(out=zt[s4][:], in_=G.zs_d[r0:r0 + 128, :]), reads=[("zs_d", r0)], writes=[("zt", s4)])

    def o_stat(t):
        s4 = t % 4; s = t % 2
        S.op("dve", lambda e: e.tensor_tensor(out=of[s4][:], in0=of[s4][:], in1=ob[s4][:], op=ALU.add), reads=[("of", s4), ("ob", s4)], writes=[("of", s4)])
        S.op("pool", lambda e: e.tensor_tensor(out=sqt[:], in0=of[s4][:], in1=of[s4][:], op=ALU.mult), reads=[("of", s4)], writes=["sqt"])
        S.op("dve", lambda e: e.tensor_reduce(out=ms[s][:], in_=sqt[:].rearrange("p (h v) -> p h v", v=128), axis=AX.X, op=ALU.add),
             reads=["sqt"], writes=[("ms", s)])
        S.op("act", lambda e: e.activation(out=ms[s][:], in_=ms[s][:], func=AF.Sqrt, bias=EPS, scale=1.0 / 128), reads=[("ms", s)], writes=[("ms", s)])
        S.op("dve", lambda e: e.reciprocal(out=ms[s][:], in_=ms[s][:]), reads=[("ms", s)], writes=[("ms", s)])

    def o_scale(t):
        s4 = t % 4; s = t % 2
        for h in range(NH):
            hs_ = slice(h * 128, (h + 1) * 128)
            S.op("dve", lambda e, h=h, hs_=hs_: e.scalar_tensor_tensor(out=of[s4][:, hs_], in0=of[s4][:, hs_], scalar=ms[s][:, h:h + 1], in1=gn[:], op0=ALU.mult, op1=ALU.mult),
                 reads=[("of", s4), ("ms", s), "gn"], writes=[("of", s4)])
        S.op("pool", lambda e: e.tensor_tensor(out=on[s][:], in0=of[s4][:], in1=zt[s4][:], op=ALU.mult), reads=[("of", s4), ("zt", s4)], writes=[("on", s)])

    def o_tr(t):
        s = t % 2; r0 = t * 128
        bk = 6 + s
        for h in range(NH):
            S.op("pe", lambda e, h=h: e.transpose(out=G.pst[s][:, h * 128:(h + 1) * 128], in_=on[s][:, h * 128:(h + 1) * 128], identity=ident[:]),
                 reads=[("on", s), "ident"], writes=[("ps", bk)])
        S.op("act", lambda e: e.activation(out=oT[s][:], in_=G.pst[s][:, :].rearrange("p (h t) -> p h t", h=8), func=AF.Copy),
             reads=[("ps", bk)], writes=[("oT", s)])
        S.dma("sp", lambda e: e.dma_start(out=G.mixT_d[GW:2 * GW, r0:r0 + 128].rearrange("(h p) t -> p h t", p=128), in_=oT[s][:]),
              reads=[("oT", s)], writes=[("mixT_d", 8 + t)])

    stages = [(0, o_load), (2, o_stat), (3, o_scale), (4, o_tr)]
    for i in range(32 + 4):
        for k_, st in stages:
            j = i - k_
            if 0 <= j < 32:
                st(j)


NE = 16
CAP = 512
FF = 1536


def declareW(G, nc, dbg=()):
    def inp(name, shape, dt=F32):
        return nc.dram_tensor(name, list(shape), dt, kind="ExternalInput").ap()
    def scr(name, shape, dt):
        kind = "ExternalOutput" if name in dbg else "Internal"
        return nc.dram_tensor(name, list(shape), dt, kind=kind).ap()
    G.w_out = inp("w_out", [D, D])
    G.w_router = inp("w_router", [D, NE])
    G.ident_f = inp("ident_f", [128, 128], F32)
    G.w_gate = inp("w_gate", [NE, D, FF])
    G.w_up = inp("w_up", [NE, D, FF])
    G.w_down = inp("w_down", [NE, FF, D])
    G.norm_f = inp("norm_f", [1, D])
    G.x1_d = scr("x1_d", [NX, D], F32)
    G.hx2_d = scr("hx2_d", [NX, D], BF16)
    G.affT_d = scr("affT_d", [NE, NX], F32)
    G.cand_d = scr("cand_d", [128, 104], F32)


def phaseW_out(G, S, es):
    nc = G.nc
    def T(name, shape, dt):
        return es.enter_context(nc.sbuf_tensor(name, shape, dt))
    wo = T("wo", [128, KC, D], BF16)
    wr = T("wr", [128, KC, NE], BF16)
    identf = T("identf", [128, 128], F32)
    gt1 = T("gt1", [128, D], F32); gm2 = T("gm2", [128, D], F32); sh2 = T("sh2", [128, D], F32)
    wov = G.w_out.rearrange("(k p) n -> p k n", p=128)
    for i in range(4):
        S.dma("pool", lambda e, i=i: e.dma_start(out=wo[:, :, i * 512:(i + 1) * 512], in_=wov[:, :, i * 512:(i + 1) * 512]), writes=[("wo", i)])
    S.dma("pool", lambda e: e.dma_start(out=wr[:], in_=G.w_router.rearrange("(k p) n -> p k n", p=128)), writes=["wr"])
    S.dma("sp", lambda e: e.dma_start(out=identf[:], in_=G.ident_f), writes=["identf"])
    S.dma("sp", lambda e: e.dma_start(out=gt1[:], in_=bc(G.mod_d[0:1, 2 * D:3 * D], D)), reads=["mod_d"], writes=["gt1"])
    S.dma("sp", lambda e: e.dma_start(out=sh2[:], in_=bc(G.mod_d[0:1, 3 * D:4 * D], D)), reads=["mod_d"], writes=["sh2"])
    S.dma("sp", lambda e: e.dma_start(out=gm2[:], in_=bc(G.mod_d[0:1, 4 * D:5 * D], D)), reads=["mod_d"], writes=["gm2"])
    mT = [T("mT%d" % i, [128, KC, 128], BF16) for i in range(2)]
    xt = [T("wxt%d" % i, [128, D], F32) for i in range(2)]
    x1 = [T("x1_%d" % i, [128, D], F32) for i in range(2)]
    tmp = T("wtmp", [128, D], F32)
    hb = [T("whb%d" % i, [128, D], BF16) for i in range(2)]
    junk = T("wjunk", [128, D], BF16)
    ss = T("wss", [128, 32], F32)
    hT = [T("whT%d" % i, [128, KC, 128], BF16) for i in range(2)]
    lg = T("lg", [128, NE], F32); mx = T("mx", [128, 1], F32); sm = T("sm", [128, 1], F32)
    aff = T("aff", [128, NE], F32)
    affT = T("affT", [NE, NX], F32)
    mv = G.mixT_d.rearrange("(k p) t -> p k t", p=128)
    ident = G.ident
    lgs = [lg, T("lg1", [128, NE], F32)]
    mxs = [mx, T("mx1", [128, 1], F32)]
    sms = [sm, T("sm1", [128, 1], F32)]
    affs = [aff, T("aff1", [128, NE], F32)]

    def s_load(t):
        s = t % 2; r0 = t * 128
        S.dma("sp", lambda e: e.dma_start(out=mT[s][:], in_=mv[:, :, r0:r0 + 128]),
              reads=[("mixT_d", x_) for x_ in range(8)] + [("mixT_d", 8 + t)], writes=[("mT", s)])
        S.dma("sp", lambda e: e.dma_start(out=xt[s][:], in_=G.x[r0:r0 + 128, :]), writes=[("wxt", s)])

    def s_main(t):
        s = t % 2; r0 = t * 128
        for cb in range(4):
            for k in range(KC):
                S.op("pe", lambda e, k=k, cb=cb: e.matmul(G.ps[cb][:, :], lhsT=mT[s][:, k, :], rhs=wo[:, k, cb * 512:(cb + 1) * 512],
                                                       start=(k == 0), stop=(k == KC - 1)),
                     reads=[("mT", s), ("wo", cb)], writes=[("ps", cb)])
            cs = slice(cb * 512, (cb + 1) * 512)
            S.op("dve", lambda e, cb=cb, cs=cs: e.tensor_tensor(out=x1[s][:, cs], in0=G.ps[cb][:, :], in1=gt1[:, cs], op=ALU.mult),
                 reads=[("ps", cb), "gt1"], writes=[("x1", s, cb)])
            S.op("pool", lambda e, cs=cs: e.tensor_tensor(out=x1[s][:, cs], in0=x1[s][:, cs], in1=xt[s][:, cs], op=ALU.add),
                 reads=[("x1", s, cb), ("wxt", s)], writes=[("x1", s, cb)])
        x1k = [("x1", s, cb) for cb in range(4)]
        S.dma("pool", lambda e: e.dma_start(out=G.x1_d[r0:r0 + 128, :], in_=x1[s][:]), reads=x1k, writes=["x1_d"])

    def s_norm(t):
        s = t % 2; r0 = t * 128
        x1k = [("x1", s, cb) for cb in range(4)]
        S.op("act", lambda e: e.activation(out=junk[:], in_=x1[s][:], func=AF.Square, scale=D ** -0.5, accum_out=ss[:, t:t + 1]),
             reads=x1k, writes=["wjunk", ("wss", t)])
        S.op("act", lambda e: e.activation(out=ss[:, t:t + 1], in_=ss[:, t:t + 1], func=AF.Sqrt, bias=EPS, scale=1.0), reads=[("wss", t)], writes=[("wss", t)])
        S.op("dve", lambda e: e.reciprocal(out=ss[:, t:t + 1], in_=ss[:, t:t + 1]), reads=[("wss", t)], writes=[("wss", t)])
        S.op("dve", lambda e: e.scalar_tensor_tensor(out=tmp[:], in0=x1[s][:], scalar=ss[:, t:t + 1], in1=gm2[:], op0=ALU.mult, op1=ALU.mult),
             reads=x1k + [("wss", t), "gm2"], writes=["wtmp"])
        S.op("pool", lambda e: e.tensor_tensor(out=hb[s][:], in0=tmp[:], in1=sh2[:], op=ALU.add), reads=["wtmp", "sh2"], writes=[("whb", s)])
        S.dma("pool", lambda e: e.dma_start(out=G.hx2_d[r0:r0 + 128, :], in_=hb[s][:]), reads=[("whb", s)], writes=["hx2_d"])

    def s_tr(t):
        s = t % 2
        for half in range(2):
            bk = 6 + half
            for j in range(8):
                k = half * 8 + j
                S.op("pe", lambda e, k=k, j=j, half=half: e.transpose(out=G.pst[half][:, j * 128:(j + 1) * 128], in_=hb[s][:, k * 128:(k + 1) * 128], identity=ident[:]),
                     reads=[("whb", s), "ident"], writes=[("ps", bk)])
            if half == 0:
                S.op("act", lambda e: e.activation(out=hT[s][:, 0:8, :], in_=G.pst[0][:, :].rearrange("p (k t) -> p k t", k=8), func=AF.Copy),
                     reads=[("ps", bk)], writes=[("whT", s, 0)])
            else:
                S.op("dve", lambda e: e.tensor_copy(out=hT[s][:, 8:16, :], in_=G.pst[1][:, :].rearrange("p (k t) -> p k t", k=8)),
                     reads=[("ps", bk)], writes=[("whT", s, 1)])

    def s_route(t):
        s = t % 2
        lg_, mx_, sm_, aff_ = lgs[s], mxs[s], sms[s], affs[s]
        for k in range(KC):
            S.op("pe", lambda e, k=k: e.matmul(G.ps[4][:, 0:NE], lhsT=hT[s][:, k, :], rhs=wr[:, k, :], start=(k == 0), stop=(k == KC - 1)),
                 reads=[("whT", s, 0), ("whT", s, 1), "wr"], writes=[("ps", 4)])
        S.op("act", lambda e: e.activation(out=lg_[:], in_=G.ps[4][:, 0:NE], func=AF.Copy), reads=[("ps", 4)], writes=[("lg", s)])
        S.op("dve", lambda e: e.tensor_reduce(out=mx_[:], in_=lg_[:], axis=AX.X, op=ALU.max), reads=[("lg", s)], writes=[("mx", s)])
        S.op("dve", lambda e: e.tensor_scalar(out=mx_[:], in0=mx_[:], scalar1=-1.0, scalar2=None, op0=ALU.mult), reads=[("mx", s)], writes=[("mx", s)])
        S.op("act", lambda e: e.activation(out=aff_[:], in_=lg_[:], func=AF.Exp, bias=mx_[:, 0:1], scale=1.0, accum_out=sm_[:, 0:1]), reads=[("lg", s), ("mx", s)], writes=[("aff", s), ("sm", s)])
        S.op("dve", lambda e: e.reciprocal(out=sm_[:], in_=sm_[:]), reads=[("sm", s)], writes=[("sm", s)])
        S.op("dve", lambda e: e.tensor_scalar(out=aff_[:], in0=aff_[:], scalar1=sm_[:, 0:1], scalar2=None, op0=ALU.mult), reads=[("aff", s), ("sm", s)], writes=[("aff", s)])

    def s_afft(t):
        s = t % 2; r0 = t * 128
        aff_ = affs[s]
        S.op("pe", lambda e: e.transpose(out=G.ps[5][0:NE, 0:128], in_=aff_[:], identity=identf[:]), reads=[("aff", s), "identf"], writes=[("ps", 5)])
        S.op("act", lambda e: e.activation(out=affT[:, r0:r0 + 128], in_=G.ps[5][0:NE, 0:128], func=AF.Copy), reads=[("ps", 5)], writes=[("affT", t)])

    stages = [s_load, s_main, s_norm, s_tr, s_route, s_afft]
    for i in range(32 + len(stages) - 1):
        for k_, st in enumerate(stages):
            j = i - k_
            if 0 <= j < 32:
                st(j)
    S.dma("sp", lambda e: e.dma_start(out=G.affT_d, in_=affT[:]), reads=[("affT", t) for t in range(32)], writes=["affT_d"])


def phaseK_topk(G, S, es, keep):
    nc = G.nc
    def T(name, shape, dt, st=es):
        return st.enter_context(nc.sbuf_tensor(name, shape, dt))
    G.idxT = T("idxT", [128, 4, NE], I32, keep)
    G.valsT = T("valsT", [128, 4, NE], F32, keep)
    NSEG = 8
    NCAND = 104
    orig = T("orig", [NE, NX], F32)
    w128 = T("w128", [128, NX // NSEG], F32)
    cv = T("cv", [128, NCAND], F32)
    c16 = T("c16", [NE, NSEG * NCAND], F32)
    vals = T("vals", [NE, CAP], F32)
    idxs = T("idxs", [NE, CAP], U32)
    idxf = T("idxf", [NE, CAP], F32)
    identf = T("identf2", [128, 128], F32)
    S.dma("sp", lambda e: e.dma_start(out=identf[:], in_=G.ident_f), writes=["identf2"])
    S.dma("sp", lambda e: e.dma_start(out=orig[:], in_=G.affT_d), reads=["affT_d"], writes=["orig"])
    S.dma("sp", lambda e: e.dma_start(out=w128[:], in_=G.affT_d.rearrange("e (s t) -> (e s) t", s=NSEG)), reads=["affT_d"], writes=["w128"])
    for r in range(NCAND // 8):
        rs = slice(r * 8, (r + 1) * 8)
        S.op("dve", lambda e, rs=rs: e.max(out=cv[:, rs], in_=w128[:]), reads=["w128"], writes=[("cv", r)])
        S.op("dve", lambda e, rs=rs: e.match_replace(out=w128[:], in_to_replace=cv[:, rs], in_values=w128[:], imm_value=-1.0),
             reads=["w128", ("cv", r)], writes=["w128"])
    S.dma("sp", lambda e: e.dma_start(out=G.cand_d, in_=cv[:]), reads=[("cv", r) for r in range(NCAND // 8)], writes=["cand_d"])
    S.dma("sp", lambda e: e.dma_start(out=c16[:], in_=G.cand_d.rearrange("(e s) j -> e (s j)", s=NSEG)), reads=["cand_d"], writes=["c16"])
    for r in range(CAP // 8):
        rs = slice(r * 8, (r + 1) * 8)
        S.op("dve", lambda e, rs=rs: e.max(out=vals[:, rs], in_=c16[:]), reads=["c16"], writes=[("vals", r)])
        S.op("dve", lambda e, rs=rs: e.match_replace(out=c16[:], in_to_replace=vals[:, rs], in_values=c16[:], imm_value=-1.0),
             reads=["c16", ("vals", r)], writes=["c16"])
    for r in range(CAP // 8):
        rs = slice(r * 8, (r + 1) * 8)
        S.op("dve", lambda e, rs=rs: e.max_index(out=idxs[:, rs], in_max=vals[:, rs], in_values=orig[:]), reads=["orig", ("vals", r)], writes=[("idxs", r)])
    allv = [("vals", r) for r in range(CAP // 8)]; alli = [("idxs", r) for r in range(CAP // 8)]
    S.op("dve", lambda e: e.tensor_copy(out=idxf[:], in_=idxs[:]), reads=alli, writes=["idxf"])
    for t in range(4):
        S.op("pe", lambda e, t=t: e.transpose(out=G.ps[0][:, t * NE:(t + 1) * NE], in_=idxf[:, t * 128:(t + 1) * 128], identity=identf[0:NE, 0:NE]),
             reads=["idxf", "identf2"], writes=[("ps", 0)])
        S.op("pe", lambda e, t=t: e.transpose(out=G.ps[1][:, t * NE:(t + 1) * NE], in_=vals[:, t * 128:(t + 1) * 128], identity=identf[0:NE, 0:NE]),
             reads=allv + ["identf2"], writes=[("ps", 1)])
    S.op("dve", lambda e: e.tensor_copy(out=G.idxT[:], in_=G.ps[0][:, 0:4 * NE].rearrange("p (t e) -> p t e", e=NE)), reads=[("ps", 0)], writes=["idxT"])
    S.op("act", lambda e: e.activation(out=G.valsT[:], in_=G.ps[1][:, 0:4 * NE].rearrange("p (t e) -> p t e", e=NE), func=AF.Copy), reads=[("ps", 1)], writes=["valsT"])


def phaseM_moe(G, S, es):
    nc = G.nc
    def T(name, shape, dt):
        return es.enter_context(nc.sbuf_tensor(name, shape, dt))
    gt2 = T("gt2", [128, D], F32)
    S.dma("sp", lambda e: e.dma_start(out=gt2[:], in_=bc(G.mod_d[0:1, 5 * D:6 * D], D)), reads=["mod_d"], writes=["gt2"])
    xg = [T("xg%d" % i, [128, D], BF16) for i in range(2)]
    xgT = [T("xgT%d" % i, [128, KC, CAP], BF16) for i in range(2)]
    wg = [T("wg%d" % i, [128, KC, 512], BF16) for i in range(2)]
    wu = [T("wu%d" % i, [128, KC, 512], BF16) for i in range(2)]
    wd = [T("wd%d" % i, [128, 12, 512], BF16) for i in range(2)]
    hid = T("hid", [128, 12, CAP], BF16)
    sg = [T("sg%d" % i, [128, CAP], F32) for i in range(2)]
    yt = [T("yt%d" % i, [128, D], F32) for i in range(4)]
    ident = G.ident
    cnt = {"nw": 0, "nd": 0, "ne": 0}

    def gather_tr(ex):
        xs = ex % 2
        for t in range(4):
            gs = (ex * 4 + t) % 2
            S.dma("pool", lambda e, gs=gs, t=t, ex=ex: e.indirect_dma_start(
                out=xg[gs][:], out_offset=None, in_=G.hx2_d[:, :],
                in_offset=bass.IndirectOffsetOnAxis(ap=G.idxT[:, t, ex:ex + 1], axis=0)),
                reads=["hx2_d", "idxT"], writes=[("xg", gs)])
            for half in range(2):
                bk = 6 + half
                for j in range(8):
                    k = half * 8 + j
                    S.op("pe", lambda e, gs=gs, k=k, j=j, half=half: e.transpose(out=G.pst[half][:, j * 128:(j + 1) * 128], in_=xg[gs][:, k * 128:(k + 1) * 128], identity=ident[:]),
                         reads=[("xg", gs), "ident"], writes=[("ps", bk)])
                if half == 0:
                    S.op("act", lambda e, xs=xs, t=t: e.activation(out=xgT[xs][:, 0:8, t * 128:(t + 1) * 128], in_=G.pst[0][:, :].rearrange("p (k t) -> p k t", k=8), func=AF.Copy),
                         reads=[("ps", bk)], writes=[("xgT", xs, t, 0)])
                else:
                    S.op("dve", lambda e, xs=xs, t=t: e.tensor_copy(out=xgT[xs][:, 8:16, t * 128:(t + 1) * 128], in_=G.pst[1][:, :].rearrange("p (k t) -> p k t", k=8)),
                         reads=[("ps", bk)], writes=[("xgT", xs, t, 1)])

    def gateup(ex):
        xs = ex % 2
        xk = [("xgT", xs, t, hh) for t in range(4) for hh in range(2)]
        gv = G.w_gate[ex].rearrange("(k p) f -> p k f", p=128)
        uv = G.w_up[ex].rearrange("(k p) f -> p k f", p=128)
        for fb in range(3):
            ws = cnt["nw"] % 2
            cnt["nw"] += 1
            S.dma("pool", lambda e, ws=ws, fb=fb, gv=gv: e.dma_start(out=wg[ws][:], in_=gv[:, :, fb * 512:(fb + 1) * 512]), writes=[("wg", ws)])
            S.dma("pool", lambda e, ws=ws, fb=fb, uv=uv: e.dma_start(out=wu[ws][:], in_=uv[:, :, fb * 512:(fb + 1) * 512]), writes=[("wu", ws)])
            for c4 in range(4):
                fc = fb * 4 + c4
                es_ = cnt["ne"] % 2
                cnt["ne"] += 1
                pg = G.ps[es_ * 2]; pu = G.ps[es_ * 2 + 1]
                for k in range(KC):
                    S.op("pe", lambda e, ws=ws, k=k, c4=c4, xs=xs, pg=pg: e.matmul(pg[:, :], lhsT=wg[ws][:, k, c4 * 128:(c4 + 1) * 128], rhs=xgT[xs][:, k, :],
                                                                               start=(k == 0), stop=(k == KC - 1)),
                         reads=[("wg", ws)] + xk, writes=[("ps", es_ * 2)])
                for k in range(KC):
                    S.op("pe", lambda e, ws=ws, k=k, c4=c4, xs=xs, pu=pu: e.matmul(pu[:, :], lhsT=wu[ws][:, k, c4 * 128:(c4 + 1) * 128], rhs=xgT[xs][:, k, :],
                                                                               start=(k == 0), stop=(k == KC - 1)),
                         reads=[("wu", ws)] + xk, writes=[("ps", es_ * 2 + 1)])
                S.op("act", lambda e, es_=es_, pg=pg: e.activation(out=sg[es_][:], in_=pg[:, :], func=AF.Silu), reads=[("ps", es_ * 2)], writes=[("sg", es_)])
                S.op("dve", lambda e, es_=es_, pu=pu, fc=fc: e.tensor_tensor(out=hid[:, fc, :], in0=sg[es_][:], in1=pu[:, :], op=ALU.mult),
                     reads=[("ps", es_ * 2 + 1), ("sg", es_)], writes=[("hid", fc)])

    def down(ex):
        dv = G.w_down[ex].rearrange("(k p) n -> p k n", p=128)
        hk = [("hid", fc) for fc in range(12)]
        for cb in range(4):
            ds = cnt["nd"] % 2
            cnt["nd"] += 1
            S.dma("pool", lambda e, ds=ds, cb=cb, dv=dv: e.dma_start(out=wd[ds][:], in_=dv[:, :, cb * 512:(cb + 1) * 512]), writes=[("wd", ds)])
            for t in range(4):
                pb = 4 + (t % 2)
                for fc in range(12):
                    S.op("pe", lambda e, ds=ds, fc=fc, t=t, pb=pb: e.matmul(G.ps[pb][:, :], lhsT=hid[:, fc, t * 128:(t + 1) * 128], rhs=wd[ds][:, fc, :],
                                                                        start=(fc == 0), stop=(fc == 11)),
                         reads=[("wd", ds)] + hk, writes=[("ps", pb)])
                cs = slice(cb * 512, (cb + 1) * 512)
                S.op("dve", lambda e, t=t, pb=pb, cs=cs, ex=ex: e.scalar_tensor_tensor(out=yt[t][:, cs], in0=G.ps[pb][:, :], scalar=G.valsT[:, t, ex:ex + 1], in1=gt2[:, cs],
                                                                               op0=ALU.mult, op1=ALU.mult),
                     reads=[("ps", pb), "valsT", "gt2"], writes=[("yt", t, cb)])

    def scatter(ex):
        for t in range(4):
            S.dma("pool", lambda e, t=t, ex=ex: e.indirect_dma_start(
                out=G.x1_d[:, :], out_offset=bass.IndirectOffsetOnAxis(ap=G.idxT[:, t, ex:ex + 1], axis=0),
                in_=yt[t][:], in_offset=None, compute_op=ALU.add),
                reads=[("yt", t, cb) for cb in range(4)] + ["idxT", "x1_d"], writes=["x1_d"])

    gather_tr(0)
    gateup(0)
    for ex in range(NE):
        if ex + 1 < NE:
            gather_tr(ex + 1)
        down(ex)
        if ex + 1 < NE:
            gateup(ex + 1)
        scatter(ex)


def phaseZ_final(G, S, es):
    nc = G.nc
    def T(name, shape, dt):
        return es.enter_context(nc.sbuf_tensor(name, shape, dt))
    nf = T("nf", [128, D], F32)
    S.dma("sp", lambda e: e.dma_start(out=nf[:], in_=bc(G.norm_f[0:1, :], D)), writes=["nf"])
    xt = [T("zx%d" % i, [128, D], F32) for i in range(2)]
    junk = T("zjunk", [128, D], BF16)
    ss = T("zss", [128, 32], F32)
    for t in range(32):
        s = t % 2
        r0 = t * 128
        S.dma("sp", lambda e, s=s, r0=r0: e.dma_start(out=xt[s][:], in_=G.x1_d[r0:r0 + 128, :]), reads=["x1_d"], writes=[("zx", s)])
        S.op("act", lambda e, s=s, t=t: e.activation(out=junk[:], in_=xt[s][:], func=AF.Square, scale=D ** -0.5, accum_out=ss[:, t:t + 1]),
             reads=[("zx", s)], writes=["zjunk", ("zss", t)])
        S.op("act", lambda e, t=t: e.activation(out=ss[:, t:t + 1], in_=ss[:, t:t + 1], func=AF.Sqrt, bias=EPS, scale=1.0), reads=[("zss", t)], writes=[("zss", t)])
        S.op("dve", lambda e, t=t: e.reciprocal(out=ss[:, t:t + 1], in_=ss[:, t:t + 1]), reads=[("zss", t)], writes=[("zss", t)])
        S.op("dve", lambda e, s=s, t=t: e.scalar_tensor_tensor(out=xt[s][:], in0=xt[s][:], scalar=ss[:, t:t + 1], in1=nf[:], op0=ALU.mult, op1=ALU.mult),
             reads=[("zx", s), ("zss", t), "nf"], writes=[("zx", s)])
        S.dma("pool", lambda e, s=s, r0=r0: e.dma_start(out=G.out[r0:r0 + 128, :], in_=xt[s][:]), reads=[("zx", s)], writes=[("out", t)])


from concourse.bass_utils import run_bass_kernel_spmd
import ml_dtypes

_DBG = ()
N_CORES = 8
_PHASES = None


def build_program(dbg=()):
    nc = bass.Bass("TRN2", target_bir_lowering=False)
    G = declare(nc, dbg)
    declareB(G, nc, dbg); declareG(G, nc, dbg); declareF(G, nc, dbg); declareW(G, nc, dbg)
    with ExitStack() as es:
        S = Sched(nc, es)
        G.ps = [es.enter_context(nc.psum_tensor("ps%d" % i, [128, 512], F32)) for i in range(8)]
        G.pst = [G.ps[6 + i][:].bitcast(BF16) for i in range(2)]
        G.psh = [G.ps[4 + i][:].bitcast(F16) for i in range(2)]
        G.ident = es.enter_context(nc.sbuf_tensor("ident", [128, 128], BF16))
        S.dma("sp", lambda e: e.dma_start(out=G.ident[:], in_=G.ident_bf), writes=["ident"])
        keep = es
        phases = [("0", lambda G, S, es: [None for _ in phase0_mod(G, S, es)]), ("A", lambda G, S, es: [None for _ in phaseA_modulate(G, S, es)]), ("B1", phaseB1_qkv), ("B2", phaseB2_fz), ("G", phaseG_gdn),
                  ("F", phaseF_fourier), ("O", phaseO_gdnout), ("W", phaseW_out), ("K", None), ("M", phaseM_moe), ("Z", phaseZ_final)]
        for name, ph in phases:
            if _PHASES is not None and name not in _PHASES:
                continue
            with ExitStack() as es2:
                if name == "K":
                    phaseK_topk(G, S, es2, keep)
                else:
                    ph(G, S, es2)
            S.barrier()
        S.final_wait("sp", list(S.res.keys()))
        S.emit()
    return nc


def make_in_maps(inputs, n_cores=8):
    f = lambda a: np.ascontiguousarray(np.asarray(a, dtype=np.float32))
    W1, W2, W3 = fourier_tables()
    conv_w = f(inputs["conv_w"])[0]
    shared = {
        "w_mod": f(inputs["w_mod"])[0], "b_mod": f(inputs["b_mod"]), "norm1_g": f(inputs["norm1_g"]), "norm2_g": f(inputs["norm2_g"]),
        "w_in": f(inputs["w_in"])[0],
        "ident_bf": np.eye(128).astype(ml_dtypes.bfloat16), "ident_h": np.eye(128).astype(np.float16),
        "ident_f": np.eye(128, dtype=np.float32), "ones_f": np.ones((128, 128), np.float32),
        "conv_wT": np.ascontiguousarray(conv_w.reshape(5, 24, 128).transpose(2, 1, 0)),
        "ab_par": np.concatenate([f(inputs["a_log"])[0].reshape(-1), f(inputs["dt_bias"])[0].reshape(-1)])[None, :].astype(np.float32),
        "cmask": gdn_masks(), "W1": W1, "W2": W2, "W3": W3, "gnw": f(inputs["gdn_norm_w"]),
        "w_out": f(inputs["w_out"])[0], "w_router": f(inputs["w_router"])[0],
        "w_gate": f(inputs["w_gate"])[0], "w_up": f(inputs["w_up"])[0], "w_down": f(inputs["w_down"])[0],
        "norm_f": f(inputs["norm_f"])[None, :],
    }
    x = f(inputs["x"]); c = f(inputs["c"]); ctx = f(inputs["ctx"]); c_ctx = f(inputs["c_ctx"])
    maps = []
    for i in range(n_cores):
        b = i % 4
        cc = np.stack([c[b], c_ctx])
        m = dict(shared)
        m["x"] = x[b]; m["ctx"] = ctx[b]
        m["ccT"] = np.ascontiguousarray(cc.reshape(2, 16, 128).transpose(2, 1, 0))
        maps.append(m)
    return maps


def kernel(**inputs):
    nc = build_program(_DBG)
    maps = make_in_maps(inputs, N_CORES)
    res = run_bass_kernel_spmd(nc, maps, core_ids=list(range(N_CORES)))
    out = np.stack([np.asarray(res.results[b]["out"], dtype=np.float32) for b in range(4)], 0)
    return out
```
